# Optimizing a Trainium2 kernel written in Bass

```python
import math
import numpy as np
import jax
import jax.numpy as jnp
from jax import lax

D_MODEL = 2048
BATCH = 4
SEQ = 4096
DEPTH = 1

HEAD_DIM = 128
MIX_WIDTH = D_MODEL
N_HEADS_NSA = MIX_WIDTH // 2 // HEAD_DIM
N_KV_NSA = 2
N_HEADS_GDN = MIX_WIDTH // 2 // HEAD_DIM
ROT_DIM = HEAD_DIM // 4
ROPE_THETA = 500000.0
CMP_LEN = 32
CMP_STRIDE = 16
SLC_LEN = 64
SLC_TOP = 16
WINDOW = 512
WIN_QBLOCK = 128
SLC_QBLOCK = 64
N_NSA_BRANCH = 3
CONV_WIDTH = 4
GDN_CHUNK = 64
N_GROUPS = 8
EXPERTS_PER_GROUP = 8
N_EXPERTS = N_GROUPS * EXPERTS_PER_GROUP
TOP_K_IN_GROUP = 2
D_EXPERT = D_MODEL // 4
MOE_ROW_BLOCK = 256
EPS = 1e-6

NSA_Q_COLS = N_HEADS_NSA * HEAD_DIM
NSA_KV_COLS = N_NSA_BRANCH * 2 * N_KV_NSA * HEAD_DIM
NSA_GATE_COLS = N_NSA_BRANCH * N_HEADS_NSA
GDN_QKV_COLS = 3 * N_HEADS_GDN * HEAD_DIM
GDN_BETA_COLS = N_HEADS_GDN
GDN_DECAY_COLS = N_HEADS_GDN
GDN_GATE_COLS = N_HEADS_GDN * HEAD_DIM
IN_SPLITS = (NSA_Q_COLS, NSA_KV_COLS, NSA_GATE_COLS, GDN_QKV_COLS, GDN_BETA_COLS, GDN_DECAY_COLS, GDN_GATE_COLS)
D_IN_PROJ = sum(IN_SPLITS)

kernel_name = "hymba_nsa_gdn_hier_moe_block"


def rmsnorm(x, w):
    xf = x.astype(jnp.float32)
    y = xf * lax.rsqrt(jnp.mean(xf * xf, axis=-1, keepdims=True) + EPS)
    return (y * w.astype(jnp.float32)).astype(x.dtype)


def l2norm(x):
    return x * lax.rsqrt(jnp.sum(x * x, axis=-1, keepdims=True) + EPS)


def masked_softmax(s, mask):
    s = jnp.where(mask, s.astype(jnp.float32), -jnp.inf)
    m = jnp.max(s, axis=-1, keepdims=True)
    m = jnp.where(jnp.isfinite(m), m, 0.0)
    p = jnp.exp(s - m)
    return p / jnp.maximum(jnp.sum(p, axis=-1, keepdims=True), 1e-30)


def rope_tables(positions):
    inv_freq = ROPE_THETA ** (-jnp.arange(0, ROT_DIM, 2, dtype=jnp.float32) / ROT_DIM)
    ang = positions.astype(jnp.float32)[..., None] * inv_freq
    return jnp.cos(ang)[:, :, None, :], jnp.sin(ang)[:, :, None, :]


def partial_rope(x, cos, sin):
    half = ROT_DIM // 2
    x1, x2, xp = x[..., :half], x[..., half:ROT_DIM], x[..., ROT_DIM:]
    rot = jnp.concatenate([x1 * cos - x2 * sin, x1 * sin + x2 * cos], axis=-1)
    return jnp.concatenate([rot.astype(x.dtype), xp], axis=-1)


def nsa_compress(a, w, pe):
    T = a.shape[1]
    n_cmp = (T - CMP_LEN) // CMP_STRIDE + 1
    idx = np.arange(n_cmp)[:, None] * CMP_STRIDE + np.arange(CMP_LEN)[None, :]
    blocks = a[:, idx]
    return jnp.einsum('bnlgd,lde->bgne', blocks + pe[None, None, :, None, :], w)


def nsa_compressed(qg, k, v, wk, pek, wv, pev):
    B, G, R, T, D = qg.shape
    kc, vc = nsa_compress(k, wk, pek), nsa_compress(v, wv, pev)
    n_cmp = kc.shape[2]
    s = jnp.einsum('bgrtd,bgnd->bgrtn', qg, kc) * (HEAD_DIM ** -0.5)
    blk_end = jnp.arange(n_cmp) * CMP_STRIDE + CMP_LEN - 1
    mask = blk_end[None, :] <= jnp.arange(T)[:, None]
    p = masked_softmax(s, mask)
    o = jnp.einsum('bgrtn,bgnd->bgrtd', p, vc)
    return o, p


def nsa_selected(qg, k, v, p_cmp):
    B, G, R, T, D = qg.shape
    n_cmp = p_cmp.shape[-1]
    n_slc = T // SLC_LEN
    n_sel = min(SLC_TOP, n_slc)
    ci = np.arange(n_cmp)[:, None] * CMP_STRIDE
    sj = np.arange(n_slc)[None, :] * SLC_LEN
    agg = jnp.asarray(((ci < sj + SLC_LEN) & (ci + CMP_LEN > sj)).astype(np.float32))
    imp = jnp.einsum('bgrtn,nm->bgtm', p_cmp, agg)
    t = jnp.arange(T)[:, None]
    j = jnp.arange(n_slc)[None, :]
    cur = t // SLC_LEN
    valid = j <= cur
    forced = (j == 0) | (j == cur) | (j == cur - 1)
    imp = jnp.where(forced, jnp.inf, jnp.where(valid, imp, -jnp.inf))
    _, sel = lax.top_k(imp, n_sel)

    kb = k.transpose(0, 2, 1, 3).reshape(B, G, n_slc, SLC_LEN, D)
    vb = v.transpose(0, 2, 1, 3).reshape(B, G, n_slc, SLC_LEN, D)
    nqb = T // SLC_QBLOCK
    q_blocks = qg.reshape(B, G, R, nqb, SLC_QBLOCK, D).transpose(3, 0, 1, 2, 4, 5)
    sel_blocks = sel.reshape(B, G, nqb, SLC_QBLOCK, n_sel).transpose(2, 0, 1, 3, 4)
    t_blocks = jnp.arange(T).reshape(nqb, SLC_QBLOCK)
    bi = jnp.arange(B)[:, None, None, None]
    gi = jnp.arange(G)[None, :, None, None]

    def one_block(args):
        qb, sb, tb = args
        ks = kb[bi, gi, sb].reshape(B, G, SLC_QBLOCK, n_sel * SLC_LEN, D)
        vs = vb[bi, gi, sb].reshape(B, G, SLC_QBLOCK, n_sel * SLC_LEN, D)
        kpos = (sb[..., None] * SLC_LEN + jnp.arange(SLC_LEN)).reshape(B, G, SLC_QBLOCK, n_sel * SLC_LEN)
        mask = (kpos <= tb[None, None, :, None])[:, :, None]
        s = jnp.einsum('bgrqd,bgqkd->bgrqk', qb, ks) * (HEAD_DIM ** -0.5)
        p = masked_softmax(s, mask)
        return jnp.einsum('bgrqk,bgqkd->bgrqd', p, vs)

    o = lax.map(one_block, (q_blocks, sel_blocks, t_blocks))
    return o.transpose(1, 2, 3, 0, 4, 5).reshape(B, G, R, T, D)


def nsa_window(qg, k, v):
    B, G, R, T, D = qg.shape
    nb = T // WIN_QBLOCK
    n_band = WINDOW // WIN_QBLOCK + 1

    def band(a):
        a = jnp.pad(a.transpose(0, 2, 1, 3), ((0, 0), (0, 0), (WINDOW, 0), (0, 0)))
        a = a.reshape(B, G, nb + n_band - 1, WIN_QBLOCK, D)
        return jnp.concatenate([a[:, :, i:i + nb] for i in range(n_band)], axis=3)

    kb, vb = band(k), band(v)
    qb = qg.reshape(B, G, R, nb, WIN_QBLOCK, D)
    qi = jnp.arange(WIN_QBLOCK)[:, None]
    kj = jnp.arange(n_band * WIN_QBLOCK)[None, :]
    dist = WINDOW + qi - kj
    kpos = jnp.arange(nb)[:, None, None] * WIN_QBLOCK - WINDOW + kj[None]
    mask = (dist >= 0) & (dist < WINDOW) & (kpos >= 0)
    s = jnp.einsum('bgrnqd,bgnkd->bgrnqk', qb, kb) * (HEAD_DIM ** -0.5)
    p = masked_softmax(s, mask)
    o = jnp.einsum('bgrnqk,bgnkd->bgrnqd', p, vb)
    return o.reshape(B, G, R, T, D)


def nsa_mixer(q_raw, kv_raw, gate_raw, cos, sin, cmp_wk, cmp_pek, cmp_wv, cmp_pev):
    B, T, _ = q_raw.shape
    H, G, D = N_HEADS_NSA, N_KV_NSA, HEAD_DIM
    R = H // G
    q = partial_rope(q_raw.reshape(B, T, H, D), cos, sin)
    kv = kv_raw.reshape(B, T, N_NSA_BRANCH, 2, G, D)
    k_c, v_c = partial_rope(kv[:, :, 0, 0], cos, sin), kv[:, :, 0, 1]
    k_s, v_s = partial_rope(kv[:, :, 1, 0], cos, sin), kv[:, :, 1, 1]
    k_w, v_w = partial_rope(kv[:, :, 2, 0], cos, sin), kv[:, :, 2, 1]
    qg = q.reshape(B, T, G, R, D).transpose(0, 2, 3, 1, 4)
    o_c, p_c = nsa_compressed(qg, k_c, v_c, cmp_wk, cmp_pek, cmp_wv, cmp_pev)
    o_s = nsa_selected(qg, k_s, v_s, p_c)
    o_w = nsa_window(qg, k_w, v_w)
    to_bthd = lambda o: o.transpose(0, 3, 1, 2, 4).reshape(B, T, H, D)
    gates = jax.nn.sigmoid(gate_raw.astype(jnp.float32)).reshape(B, T, H, N_NSA_BRANCH)
    o = (gates[..., 0:1] * to_bthd(o_c) + gates[..., 1:2] * to_bthd(o_s)
         + gates[..., 2:3] * to_bthd(o_w))
    return o.reshape(B, T, H * D).astype(q_raw.dtype)


def causal_depthwise_conv(x, w):
    C = x.shape[-1]
    return lax.conv_general_dilated(x, w[:, None, :].astype(x.dtype), window_strides=(1,),
                                    padding=[(CONV_WIDTH - 1, 0)],
                                    dimension_numbers=('NWC', 'WIO', 'NWC'),
                                    feature_group_count=C)


def chunked_gated_delta(q, k, v, g, beta):
    B, H, T, Dk = q.shape
    Dv = v.shape[-1]
    C = GDN_CHUNK
    N = T // C
    q = q * (Dk ** -0.5)
    kb = k * beta[..., None]
    vb = v * beta[..., None]
    rs = lambda a: a.reshape(B, H, N, C, a.shape[-1])
    q, k, v, kb, vb = rs(q), rs(k), rs(v), rs(kb), rs(vb)
    g = jnp.cumsum(g.reshape(B, H, N, C), axis=-1)
    tril = jnp.tril(jnp.ones((C, C), dtype=bool))
    strict = jnp.tril(jnp.ones((C, C), dtype=bool), -1)
    decay = jnp.exp(jnp.where(tril, g[..., :, None] - g[..., None, :], -jnp.inf))
    lower = jnp.where(strict, jnp.einsum('bhncd,bhnsd->bhncs', kb, k) * decay, 0.0)
    eye = jnp.eye(C, dtype=q.dtype)
    t_inv = lax.linalg.triangular_solve(eye + lower, jnp.broadcast_to(eye, lower.shape),
                                        left_side=True, lower=True, unit_diagonal=True)
    u = t_inv @ vb
    w = t_inv @ (kb * jnp.exp(g)[..., None])
    attn = jnp.where(tril, jnp.einsum('bhncd,bhnsd->bhncs', q, k) * decay, 0.0)
    q_dec = q * jnp.exp(g)[..., None]
    k_dec = k * jnp.exp(g[..., -1:] - g)[..., None]
    g_last = jnp.exp(g[..., -1])

    def step(S, xs):
        u_i, w_i, a_i, q_i, k_i, gl_i = xs
        v_new = u_i - w_i @ S
        o_i = q_i @ S + a_i @ v_new
        S = S * gl_i[..., None, None] + jnp.swapaxes(k_i, -1, -2) @ v_new
        return S, o_i

    front = lambda a: jnp.moveaxis(a, 2, 0)
    S0 = jnp.zeros((B, H, Dk, Dv), q.dtype)
    _, o = lax.scan(step, S0, (front(u), front(w), front(attn), front(q_dec), front(k_dec), front(g_last)))
    return o.transpose(1, 2, 0, 3, 4).reshape(B, H, T, Dv)


def gdn_mixer(qkv_raw, b_raw, a_raw, z_raw, conv_w, a_log, dt_bias, norm_w):
    B, T, _ = qkv_raw.shape
    H, D = N_HEADS_GDN, HEAD_DIM
    qkv = jax.nn.silu(causal_depthwise_conv(qkv_raw, conv_w)).astype(jnp.float32)
    q, k, v = [a.reshape(B, T, H, D).transpose(0, 2, 1, 3) for a in jnp.split(qkv, 3, axis=-1)]
    q, k = l2norm(q), l2norm(k)
    beta = jax.nn.sigmoid(b_raw.astype(jnp.float32)).transpose(0, 2, 1)
    g = (-jnp.exp(a_log.astype(jnp.float32))
         * jax.nn.softplus(a_raw.astype(jnp.float32) + dt_bias.astype(jnp.float32))).transpose(0, 2, 1)
    o = chunked_gated_delta(q, k, v, g, beta).transpose(0, 2, 1, 3)
    o = rmsnorm(o, norm_w) * jax.nn.silu(z_raw.reshape(B, T, H, D).astype(jnp.float32))
    return o.reshape(B, T, H * D).astype(qkv_raw.dtype)


def hier_moe(h, wr_grp, br_grp, wr_exp, br_exp, w_gate, w_up, w_down):
    B, T, D = h.shape
    N = B * T
    hf = h.reshape(N, D)
    grp_logits = (hf @ wr_grp).astype(jnp.float32) + br_grp.astype(jnp.float32)
    grp_prob = jax.nn.softmax(grp_logits, axis=-1)
    grp = jnp.argmax(grp_logits, axis=-1)
    p_grp = jnp.take_along_axis(grp_prob, grp[:, None], axis=-1)[:, 0]
    exp_logits = ((hf @ wr_exp).astype(jnp.float32) + br_exp.astype(jnp.float32)).reshape(N, N_GROUPS, EXPERTS_PER_GROUP)
    in_grp = jnp.take_along_axis(exp_logits, grp[:, None, None], axis=1)[:, 0]
    top_p, top_i = lax.top_k(jax.nn.softmax(in_grp, axis=-1), TOP_K_IN_GROUP)
    weights = top_p / jnp.sum(top_p, axis=-1, keepdims=True) * p_grp[:, None]
    expert = grp[:, None] * EXPERTS_PER_GROUP + top_i

    M = N * TOP_K_IN_GROUP
    Rb = MOE_ROW_BLOCK
    e_flat = expert.reshape(M)
    w_flat = weights.reshape(M)
    tok_flat = jnp.repeat(jnp.arange(N, dtype=jnp.int32), TOP_K_IN_GROUP)
    order = jnp.argsort(e_flat)
    e_sorted = e_flat[order]
    counts = jnp.zeros((N_EXPERTS,), jnp.int32).at[e_flat].add(1)
    padded = (counts + Rb - 1) // Rb * Rb
    pad_end = jnp.cumsum(padded)
    pad_start = pad_end - padded
    start = jnp.cumsum(counts) - counts
    dest = pad_start[e_sorted] + (jnp.arange(M, dtype=jnp.int32) - start[e_sorted])
    n_blocks = (M + N_EXPERTS * (Rb - 1) + Rb - 1) // Rb
    P = n_blocks * Rb
    row_tok = jnp.full((P,), N, jnp.int32).at[dest].set(tok_flat[order])
    row_w = jnp.zeros((P,), jnp.float32).at[dest].set(w_flat[order])
    blk_expert = jnp.minimum(jnp.searchsorted(pad_end, jnp.arange(n_blocks, dtype=jnp.int32) * Rb, side='right'),
                             N_EXPERTS - 1)
    h_pad = jnp.concatenate([hf, jnp.zeros((1, D), hf.dtype)], axis=0)

    def expert_block(args):
        tok, e, wt = args
        xb = h_pad[tok]
        y = (jax.nn.silu(xb @ w_gate[e]) * (xb @ w_up[e])) @ w_down[e]
        return y * wt[:, None].astype(y.dtype)

    y = lax.map(expert_block, (row_tok.reshape(n_blocks, Rb), blk_expert, row_w.reshape(n_blocks, Rb)))
    out = jnp.zeros((N + 1, D), y.dtype).at[row_tok].add(y.reshape(P, D))[:N]
    return out.reshape(B, T, D).astype(h.dtype)


def setup_inputs(seed: int = 0) -> dict:
    key = jax.random.key(seed)
    ks = jax.random.split(key, 24)
    f32 = jnp.float32
    nrm = lambda k, shape, scale: jax.random.normal(k, shape, f32) * scale
    L = DEPTH
    x = nrm(ks[0], (BATCH, SEQ, D_MODEL), 1.0)
    positions = jnp.broadcast_to(jnp.arange(SEQ, dtype=jnp.int32), (BATCH, SEQ))
    attn_norm_w = 1.0 + nrm(ks[1], (L, D_MODEL), 0.02)
    w_in = nrm(ks[2], (L, D_MODEL, D_IN_PROJ), D_MODEL ** -0.5)
    cmp_wk = nrm(ks[3], (L, CMP_LEN, HEAD_DIM, HEAD_DIM), (CMP_LEN * HEAD_DIM) ** -0.5)
    cmp_pek = nrm(ks[4], (L, CMP_LEN, HEAD_DIM), 0.1)
    cmp_wv = nrm(ks[5], (L, CMP_LEN, HEAD_DIM, HEAD_DIM), (CMP_LEN * HEAD_DIM) ** -0.5)
    cmp_pev = nrm(ks[6], (L, CMP_LEN, HEAD_DIM), 0.1)
    gdn_conv_w = nrm(ks[7], (L, CONV_WIDTH, GDN_QKV_COLS), CONV_WIDTH ** -0.5)
    gdn_a_log = jnp.log(jax.random.uniform(ks[8], (L, N_HEADS_GDN), f32, 1.0, 16.0))
    dt = jnp.exp(jax.random.uniform(ks[9], (L, N_HEADS_GDN), f32, math.log(1e-3), math.log(1e-1)))
    gdn_dt_bias = dt + jnp.log(-jnp.expm1(-dt))
    gdn_norm_w = 1.0 + nrm(ks[10], (L, HEAD_DIM), 0.02)
    w_out = nrm(ks[11], (L, MIX_WIDTH, D_MODEL), MIX_WIDTH ** -0.5)
    ffn_norm_w = 1.0 + nrm(ks[12], (L, D_MODEL), 0.02)
    router_group_w = nrm(ks[13], (L, D_MODEL, N_GROUPS), D_MODEL ** -0.5)
    router_group_b = nrm(ks[14], (L, N_GROUPS), 0.01)
    router_expert_w = nrm(ks[15], (L, D_MODEL, N_EXPERTS), D_MODEL ** -0.5)
    router_expert_b = nrm(ks[16], (L, N_EXPERTS), 0.01)
    moe_w_gate = nrm(ks[17], (L, N_EXPERTS, D_MODEL, D_EXPERT), D_MODEL ** -0.5)
    moe_w_up = nrm(ks[18], (L, N_EXPERTS, D_MODEL, D_EXPERT), D_MODEL ** -0.5)
    moe_w_down = nrm(ks[19], (L, N_EXPERTS, D_EXPERT, D_MODEL), D_EXPERT ** -0.5)
    final_norm_w = 1.0 + nrm(ks[20], (D_MODEL,), 0.02)
    return {"x": x, "positions": positions, "attn_norm_w": attn_norm_w, "w_in": w_in,
            "cmp_wk": cmp_wk, "cmp_pek": cmp_pek, "cmp_wv": cmp_wv, "cmp_pev": cmp_pev,
            "gdn_conv_w": gdn_conv_w, "gdn_a_log": gdn_a_log, "gdn_dt_bias": gdn_dt_bias,
            "gdn_norm_w": gdn_norm_w, "w_out": w_out, "ffn_norm_w": ffn_norm_w,
            "router_group_w": router_group_w, "router_group_b": router_group_b,
            "router_expert_w": router_expert_w, "router_expert_b": router_expert_b,
            "moe_w_gate": moe_w_gate, "moe_w_up": moe_w_up, "moe_w_down": moe_w_down,
            "final_norm_w": final_norm_w}


def reference(x, positions, attn_norm_w, w_in, cmp_wk, cmp_pek, cmp_wv, cmp_pev,
              gdn_conv_w, gdn_a_log, gdn_dt_bias, gdn_norm_w, w_out, ffn_norm_w,
              router_group_w, router_group_b, router_expert_w, router_expert_b,
              moe_w_gate, moe_w_up, moe_w_down, final_norm_w):
    cos, sin = rope_tables(positions)
    offsets = []
    acc = 0
    for n in IN_SPLITS[:-1]:
        acc += n
        offsets.append(acc)
    h = x
    for l in range(DEPTH):
        hn = rmsnorm(h, attn_norm_w[l])
        proj = hn @ w_in[l]
        nsa_q, nsa_kv, nsa_gate, gdn_qkv, gdn_b, gdn_a, gdn_z = jnp.split(proj, offsets, axis=-1)
        o_a = nsa_mixer(nsa_q, nsa_kv, nsa_gate, cos, sin, cmp_wk[l], cmp_pek[l], cmp_wv[l], cmp_pev[l])
        o_b = gdn_mixer(gdn_qkv, gdn_b, gdn_a, gdn_z, gdn_conv_w[l], gdn_a_log[l], gdn_dt_bias[l], gdn_norm_w[l])
        h = h + jnp.concatenate([o_a, o_b], axis=-1) @ w_out[l]
        h = h + hier_moe(rmsnorm(h, ffn_norm_w[l]), router_group_w[l], router_group_b[l],
                         router_expert_w[l], router_expert_b[l],
                         moe_w_gate[l], moe_w_up[l], moe_w_down[l])
    return rmsnorm(h, final_norm_w)
```

```python
from contextlib import ExitStack
import numpy as np
import concourse.bass as bass
import concourse.mybir as mybir
from concourse.bass_utils import run_bass_kernel_spmd

F32 = mybir.dt.float32
BF16 = mybir.dt.bfloat16
I32 = mybir.dt.int32
U32 = mybir.dt.uint32
ALU = mybir.AluOpType
AF = mybir.ActivationFunctionType
AX = mybir.AxisListType

D = 2048
KC = D // 128
HD = 128
NEG = -30000.0
EPS = 1e-6


class Buf:
    __slots__ = ("name", "w", "r", "dsem", "dcnt", "ap", "is_dram")

    def __init__(self, name, ap=None, is_dram=False):
        self.name = name
        self.w = None
        self.r = {}
        self.dsem = None
        self.dcnt = 0
        self.ap = ap
        self.is_dram = is_dram

    def __getitem__(self, k):
        return self.ap[k]


class Sched:
    def __init__(self, nc, stack):
        self.nc = nc
        self.stack = stack
        self.eng = {"pe": nc.tensor, "act": nc.scalar, "dve": nc.vector,
                    "pool": nc.gpsimd, "sp": nc.sync}
        self.sem = {}
        self.cnt = {}
        for e in ("pe", "act", "dve", "pool"):
            self.sem[e] = stack.enter_context(nc.semaphore("s_" + e))
            self.cnt[e] = 0
        self.waited = {}
        self.n_ins = 0
        self.n_dsem = 0
        self.dbufs = []

    def sbuf(self, name, shape, dtype, stack=None):
        st = stack if stack is not None else self.stack
        t = st.enter_context(self.nc.sbuf_tensor(name, list(shape), dtype))
        return Buf(name, t.ap())

    def psum(self, name, shape, dtype, stack=None):
        st = stack if stack is not None else self.stack
        t = st.enter_context(self.nc.psum_tensor(name, list(shape), dtype))
        return Buf(name, t.ap())

    def dram(self, name, shape, dtype, kind="Internal"):
        t = self.nc.dram_tensor(name, list(shape), dtype, kind=kind)
        return Buf(name, t.ap(), is_dram=True)

    def _wait(self, e, ev):
        if ev[0] == "eng":
            sem, val = self.sem[ev[1]], ev[2]
        else:
            sem, val = ev[1].dsem, ev[1].dcnt
        key = (e, id(sem))
        if self.waited.get(key, 0) >= val:
            return
        self.waited[key] = val
        self.eng[e].wait_ge(sem, val)

    def _deps(self, e, reads, writes, dma=False):
        evs = []
        for b in reads:
            if b.w is not None:
                evs.append(b.w)
        for b in writes:
            if b.w is not None:
                evs.append(b.w)
            for ev in b.r.values():
                evs.append(ev)
        for ev in evs:
            if (not dma) and ev[0] == "eng" and ev[1] == e and e == "pe":
                continue
            self._wait(e, ev)

    def op(self, e, fn, reads=(), writes=()):
        self._deps(e, reads, writes)
        ins = fn(self.eng[e])
        self.cnt[e] += 1
        ins.then_inc(self.sem[e], 1)
        ev = ("eng", e, self.cnt[e])
        for b in writes:
            b.w = ev
            b.r = {}
        for b in reads:
            if b not in writes:
                b.r[e] = ev
        self.n_ins += 1
        return ins

    def dma(self, q, out_ap, in_ap, out_buf, in_buf, extra_reads=(), fn=None):
        sb = out_buf if not out_buf.is_dram else in_buf
        assert not sb.is_dram
        if sb.dsem is None:
            sb.dsem = self.stack.enter_context(self.nc.semaphore("d%d" % self.n_dsem))
            self.n_dsem += 1
            self.dbufs.append(sb)
        self._deps(q, [in_buf] + list(extra_reads), [out_buf], dma=True)
        if fn is None:
            ins = self.eng[q].dma_start(out=out_ap, in_=in_ap)
        else:
            ins = fn(self.eng[q])
        ins.then_inc(sb.dsem, 16)
        sb.dcnt += 16
        ev = ("dma", sb)
        out_buf.w = ev
        out_buf.r = {}
        in_buf.r["dma%d" % id(sb)] = ev
        for b in extra_reads:
            b.r["dma%d" % id(sb)] = ev
        self.n_ins += 1
        return ins

    def barrier(self):
        for e in ("sp", "pool", "act", "dve", "pe"):
            for e2 in ("pe", "act", "dve", "pool"):
                if e2 != e and self.cnt[e2] > 0:
                    self._wait(e, ("eng", e2, self.cnt[e2]))
            for b in self.dbufs:
                self._wait(e, ("dma", b))

    def wait_all(self, e, bufs):
        for b in bufs:
            if b.w is not None:
                self._wait(e, b.w)
            for ev in b.r.values():
                self._wait(e, ev)


class Prog:
    pass


def build_consts(S, P, st):
    c = Prog()
    c.identf = S.sbuf("identf", [128, 128], F32, st)
    c.ident = S.sbuf("ident", [128, 128], BF16, st)
    S.op("pool", lambda e: e.memset(c.identf[:], 0.0), writes=[c.identf])
    S.op("pool", lambda e: e.affine_select(c.identf[:], c.identf[:], [[-1, 128]], ALU.not_equal, 1.0,
                                           base=0, channel_multiplier=1),
         reads=[c.identf], writes=[c.identf])
    S.op("dve", lambda e: e.tensor_copy(c.ident[:], c.identf[:]), reads=[c.identf], writes=[c.ident])
    return c


def phase_a0(S, P, c, ps):
    T, NT = P.T, P.NT
    with ExitStack() as st:
        xs = [S.sbuf("a0_x%d" % i, [128, D], F32, st) for i in range(2)]
        junk = S.sbuf("a0_junk", [128, D], BF16, st)
        xh = [S.sbuf("a0_xh%d" % i, [128, D], BF16, st) for i in range(2)]
        xt = [S.sbuf("a0_xt%d" % i, [128, KC, 128], BF16, st) for i in range(2)]
        ss = S.sbuf("a0_ss", [128, 2], F32, st)
        if hasattr(P, "xdisp"):
            zt = S.sbuf("a0_zt", [128, D], BF16, st)
            S.op("pool", lambda e: e.memset(zt[:], 0.0), writes=[zt])
            for r0 in range(0, 64 * 128, 128):
                S.dma("pool", P.xdisp.ap[r0:r0 + 128, :], zt[:], P.xdisp, zt)
        S.dma("sp", xs[0][:], P.x[0:128, :], xs[0], P.x)
        for i in range(NT):
            b = i % 2
            if i + 1 < NT:
                S.dma("sp", xs[1 - b][:], P.x[(i + 1) * 128:(i + 2) * 128, :], xs[1 - b], P.x)
            X = xs[b]
            S.op("act", lambda e: e.activation(junk[:], X[:], AF.Square, accum_out=ss[:, 0:1]),
                 reads=[X], writes=[junk, ss])
            S.op("act", lambda e: e.activation(ss[:, 1:2], ss[:, 0:1], AF.Sqrt, bias=c.eps[:, 0:1], scale=1.0 / D),
                 reads=[ss, c.eps], writes=[ss])
            S.op("dve", lambda e: e.reciprocal(ss[:, 1:2], ss[:, 1:2]), reads=[ss], writes=[ss])
            S.op("dve", lambda e: e.tensor_scalar(xh[b][:], X[:], ss[:, 1:2], None, ALU.mult),
                 reads=[X, ss], writes=[xh[b]])
            for half in range(2):
                pb = ps[half]
                pv = pb.ap.bitcast(BF16)
                for j in range(8):
                    k = half * 8 + j
                    S.op("pe", lambda e: e.transpose(pv[:, j * 128:(j + 1) * 128], xh[b][:, k * 128:(k + 1) * 128], c.ident[:]),
                         reads=[xh[b], c.ident], writes=[pb])
                eng = "act" if half == 0 else "dve"
                if eng == "act":
                    S.op("act", lambda e: e.copy(xt[b][:, half * 8:(half + 1) * 8, :], pv[:, 0:1024].rearrange("p (k t) -> p k t", k=8)),
                         reads=[pb], writes=[xt[b]])
                else:
                    S.op("dve", lambda e: e.tensor_copy(xt[b][:, half * 8:(half + 1) * 8, :], pv[:, 0:1024].rearrange("p (k t) -> p k t", k=8)),
                         reads=[pb], writes=[xt[b]])
            S.dma("pool", P.xT[:, :, i * 128:(i + 1) * 128], xt[b][:], P.xT, xt[b])


def rope_tables(S, P, c, st):
    T = P.T
    cosT = S.sbuf("rp_cos", [32, T], F32, st)
    sinT = S.sbuf("rp_sin", [32, T], F32, st)
    st2 = ExitStack()
    posi = S.sbuf("rp_posi", [32, T], I32, st2)
    ang = S.sbuf("rp_ang", [32, T], F32, st2)
    tmp = S.sbuf("rp_tmp", [32, T], F32, st2)
    ki = S.sbuf("rp_ki", [32, T], I32, st2)
    S.dma("sp", posi[:], P.pos.ap.partition_broadcast(32), posi, P.pos)
    S.op("dve", lambda e: e.tensor_copy(ang[:], posi[:]), reads=[posi], writes=[ang])
    S.op("dve", lambda e: e.tensor_scalar(ang[:], ang[:], c.invf[0:32, 0:1], None, ALU.mult), reads=[ang, c.invf], writes=[ang])
    TWO_PI = 2.0 * np.pi

    def reduced_sin(out, shift, sign_ap):
        S.op("dve", lambda e: e.tensor_scalar(tmp[:], ang[:], 1.0 / TWO_PI, (shift / TWO_PI) + 0.5, ALU.mult, ALU.add), reads=[ang], writes=[tmp])
        S.op("dve", lambda e: e.tensor_copy(ki[:], tmp[:]), reads=[tmp], writes=[ki])
        S.op("dve", lambda e: e.tensor_copy(tmp[:], ki[:]), reads=[ki], writes=[tmp])
        S.op("dve", lambda e: e.scalar_tensor_tensor(tmp[:], tmp[:], -TWO_PI, ang[:], ALU.mult, ALU.add), reads=[tmp, ang], writes=[tmp])
        S.op("dve", lambda e: e.tensor_scalar(tmp[:], tmp[:], float(shift), None, ALU.add), reads=[tmp], writes=[tmp])
        S.op("dve", lambda e: e.tensor_scalar(out[:], tmp[:], float(np.pi), -TWO_PI, ALU.is_gt, ALU.mult), reads=[tmp], writes=[out])
        S.op("dve", lambda e: e.tensor_tensor(tmp[:], tmp[:], out[:], ALU.add), reads=[tmp, out], writes=[tmp])
        S.op("dve", lambda e: e.tensor_scalar(out[:], tmp[:], float(-np.pi), TWO_PI, ALU.is_lt, ALU.mult), reads=[tmp], writes=[out])
        S.op("dve", lambda e: e.tensor_tensor(tmp[:], tmp[:], out[:], ALU.add), reads=[tmp, out], writes=[tmp])
        S.op("dve", lambda e: e.tensor_scalar(tmp[:], tmp[:], float(np.pi), float(-np.pi), ALU.min, ALU.max), reads=[tmp], writes=[tmp])
        S.op("act", lambda e: e.activation(out[:], tmp[:], AF.Sin), reads=[tmp], writes=[out])
        if sign_ap is not None:
            S.op("dve", lambda e: e.tensor_scalar(out[:], out[:], sign_ap, None, ALU.mult), reads=[out, c.sgn], writes=[out])

    reduced_sin(sinT, 0.0, c.sgn[0:32, 0:1])
    reduced_sin(cosT, np.pi / 2.0, None)
    S.barrier()
    st2.close()
    return cosT, sinT


def phase_a1(S, P, c, ps):
    T, NT = P.T, P.NT
    NCH = T // 512
    with ExitStack() as st:
        cosT, sinT = rope_tables(S, P, c, st)
        WbL = [S.sbuf("a1_wb%d" % i, [128, KC, 1536], BF16, st) for i in range(2)]
        gstate = {"gi": 0}
        Wf = [S.sbuf("a1_wf%d" % i, [128, KC, 128], F32, st) for i in range(2)]
        xtc = [S.sbuf("a1_xt%d" % i, [128, KC, 512], BF16, st) for i in range(2)]
        qf = [S.sbuf("a1_qf%d" % i, [128, 512], BF16, st) for i in range(2)]
        t1 = S.sbuf("a1_t1", [32, 512], F32, st)
        t2 = S.sbuf("a1_t2", [32, 512], F32, st)
        xc = [S.sbuf("a1_xc%d" % i, [128, 515], F32, st) for i in range(2)]
        yc = [S.sbuf("a1_yc%d" % i, [128, 512], F32, st) for i in range(2)]
        carry = S.sbuf("a1_carry", [128, 24, 3], F32, st)
        tmo = [S.sbuf("a1_tmo%d" % i, [128, 512], F32, st) for i in range(2)]
        tmb = [S.sbuf("a1_tmb%d" % i, [128, 512], BF16, st) for i in range(2)]
        S.op("pool", lambda e: e.memset(carry[:], 0.0), writes=[carry])
        wcnt = [0]
        rr = [0]

        def w_pieces(col0, ncols):
            return [(col0, j0, min(128, ncols - j0)) for j0 in range(0, ncols, 128)]

        def load_piece(Wb, col0, j0, n):
            wf = Wf[wcnt[0] % 2]
            wcnt[0] += 1
            S.dma("sp", wf[:, :, 0:n], P.w_in.ap[:, col0 + j0:col0 + j0 + n].rearrange("(k p) n -> p k n", p=128), wf, P.w_in)
            S.op("pool", lambda e: e.tensor_tensor(Wb[:, :, j0:j0 + n], wf[:, :, 0:n],
                                                   c.anw[:, :].unsqueeze(2).to_broadcast([128, KC, n]), ALU.mult),
                 reads=[wf, c.anw], writes=[Wb])

        def load_x(ch, b):
            S.dma("sp", xtc[b][:], P.xT[:, :, ch * 512:(ch + 1) * 512], xtc[b], P.xT)

        def fm_mm(pb, j0, xb):
            Wb = gstate["Wb"]
            for k in range(KC):
                S.op("pe", lambda e: e.matmul(pb[:, :], Wb[:, k, j0:j0 + 128], xb[:, k, :], start=(k == 0), stop=(k == KC - 1)),
                     reads=[Wb, xb], writes=[pb])

        def group(col0, ncols, blocks, nxt=None):
            gi = gstate["gi"]
            gstate["gi"] += 1
            Wb = WbL[gi % 2]
            gstate["Wb"] = Wb
            if gi == 0:
                for pc_ in w_pieces(col0, ncols):
                    load_piece(Wb, *pc_)
            pend = w_pieces(*nxt) if nxt is not None else []
            per = (len(pend) + NCH - 1) // NCH if pend else 0
            load_x(0, 0)
            for ch in range(NCH):
                b = ch % 2
                if ch + 1 < NCH:
                    load_x(ch + 1, 1 - b)
                for _ in range(per):
                    if pend:
                        load_piece(WbL[(gi + 1) % 2], *pend.pop(0))
                xb = xtc[b]
                tsl = slice(ch * 512, (ch + 1) * 512)
                for blk in blocks:
                    kind = blk[0]
                    r = rr[0] % 2
                    rr[0] += 1
                    pb = ps[2 + r]
                    if kind == "rope":
                        _, j0, dst = blk
                        fm_mm(pb, j0, xb)
                        q = qf[r]
                        S.op("act", lambda e: e.copy(q[:], pb[:, :]), reads=[pb], writes=[q])
                        pr = ps[4 + r]
                        S.op("pe", lambda e: e.matmul(pr[0:32, :], c.pswap[:, :], q[:], start=True, stop=True),
                             reads=[c.pswap, q], writes=[pr])
                        S.op("dve", lambda e: e.tensor_tensor(t1[:], pr[0:32, :], sinT[:, tsl], ALU.mult), reads=[pr, sinT], writes=[t1])
                        S.op("dve", lambda e: e.tensor_tensor(t2[:], pb[0:32, :], cosT[:, tsl], ALU.mult), reads=[pb, cosT], writes=[t2])
                        S.op("dve", lambda e: e.tensor_tensor(q[0:32, :], t1[:], t2[:], ALU.add), reads=[t1, t2], writes=[q])
                        S.dma("pool", dst[:, tsl], q[:], dst.buf, q)
                    elif kind == "fm":
                        _, j0, dst = blk
                        fm_mm(pb, j0, xb)
                        q = qf[r]
                        S.op("act", lambda e: e.copy(q[:], pb[:, :]), reads=[pb], writes=[q])
                        S.dma("pool", dst[:, tsl], q[:], dst.buf, q)
                    elif kind == "gdn":
                        _, j0, gb, dst = blk
                        fm_mm(pb, j0, xb)
                        X = xc[r]
                        Y = yc[r]
                        S.op("act", lambda e: e.copy(X[:, 0:3], carry[:, gb, :]), reads=[carry], writes=[X])
                        S.op("act", lambda e: e.copy(X[:, 3:515], pb[:, :]), reads=[pb], writes=[X])
                        S.op("act", lambda e: e.copy(carry[:, gb, :], X[:, 512:515]), reads=[X], writes=[carry])
                        S.op("dve", lambda e: e.tensor_scalar(Y[:], X[:, 0:512], c.convw[:, gb, 0:1], None, ALU.mult), reads=[X, c.convw], writes=[Y])
                        for j in range(1, 4):
                            S.op("dve", lambda e: e.scalar_tensor_tensor(Y[:], X[:, j:j + 512], c.convw[:, gb, j:j + 1], Y[:], ALU.mult, ALU.add),
                                 reads=[X, c.convw, Y], writes=[Y])
                        q = qf[r]
                        S.op("act", lambda e: e.activation(q[:], Y[:], AF.Silu), reads=[Y], writes=[q])
                        S.dma("pool", dst[:, tsl], q[:], dst.buf, q)
                    elif kind == "tm":
                        _, j0, n, post, dst = blk
                        for tt in range(4):
                            r2 = rr[0] % 2
                            rr[0] += 1
                            pb2 = ps[2 + r2]
                            for k in range(KC):
                                S.op("pe", lambda e: e.matmul(pb2[:, 0:n], xb[:, k, tt * 128:(tt + 1) * 128], Wb[:, k, j0:j0 + n],
                                                              start=(k == 0), stop=(k == KC - 1)),
                                     reads=[Wb, xb], writes=[pb2])
                            rows = slice(ch * 512 + tt * 128, ch * 512 + (tt + 1) * 128)
                            if post == "bf16":
                                o = tmb[r2]
                                S.op("act", lambda e: e.copy(o[:, 0:n], pb2[:, 0:n]), reads=[pb2], writes=[o])
                            elif post == "silu":
                                o = tmb[r2]
                                S.op("act", lambda e: e.activation(o[:, 0:n], pb2[:, 0:n], AF.Silu), reads=[pb2], writes=[o])
                            else:
                                o = tmo[r2]
                                S.op("act", lambda e: e.copy(o[:, 0:n], pb2[:, 0:n]), reads=[pb2], writes=[o])
                            S.dma("pool", dst[rows, :], o[:, 0:n], dst.buf, o)

        class V:
            def __init__(self, buf, ap):
                self.buf, self.ap = buf, ap

            def __getitem__(self, k):
                return self.ap[k]

        glist = []
        glist.append((0, 1024, [("rope", h * 128, V(P.qT, P.qT.ap[h])) for h in range(8)]))
        blocks = []
        for br in range(3):
            for g in range(2):
                blocks.append(("rope", ((br * 2 + 0) * 2 + g) * 128, V(P.kT, P.kT.ap[br * 2 + g])))
        for g in range(2):
            blocks.append(("fm", ((0 * 2 + 1) * 2 + g) * 128, V(P.vcT, P.vcT.ap[g])))
        blocks.append(("tm", 768, 256, "bf16", V(P.vs, P.vs.ap)))
        blocks.append(("tm", 1280, 256, "bf16", V(P.vw, P.vw.ap)))
        glist.append((1024, 1536, blocks))
        for gi in range(3):
            glist.append((2584 + gi * 1024, 1024,
                          [("gdn", j * 128, gi * 8 + j, V(P.gT, P.gT.ap[gi * 8 + j])) for j in range(8)]))
        glist.append((5672, 1024, [("tm", 0, 512, "silu", V(P.zs, P.zs.ap[:, 0:512])), ("tm", 512, 512, "silu", V(P.zs, P.zs.ap[:, 512:1024]))]))
        glist.append((2560, 24, [("tm", 0, 24, "f32", V(P.gate, P.gate.ap))]))
        glist.append((5656, 16, [("tm", 0, 16, "f32", V(P.bd, P.bd.ap))]))
        for gi_, (c0_, n_, bl_) in enumerate(glist):
            nxt = (glist[gi_ + 1][0], glist[gi_ + 1][1]) if gi_ + 1 < len(glist) else None
            group(c0_, n_, bl_, nxt)


def more_consts(S, P, c, st):
    c.eps = S.sbuf("c_eps", [128, 1], F32, st)
    S.op("pool", lambda e: e.memset(c.eps[:], EPS), writes=[c.eps])
    pi_ = S.sbuf("c_pi", [128, 1], I32, st)
    pf = S.sbuf("c_pf", [128, 1], F32, st)
    ge = S.sbuf("c_ge", [128, 1], F32, st)
    c.invf = S.sbuf("c_invf", [128, 1], F32, st)
    c.sgn = S.sbuf("c_sgn", [128, 1], F32, st)
    S.op("pool", lambda e: e.iota(pi_[:], [[0, 1]], base=0, channel_multiplier=1), writes=[pi_])
    S.op("dve", lambda e: e.tensor_copy(pf[:], pi_[:]), reads=[pi_], writes=[pf])
    S.op("dve", lambda e: e.tensor_scalar(ge[:], pf[:], 16.0, None, ALU.is_ge), reads=[pf], writes=[ge])
    S.op("dve", lambda e: e.tensor_scalar(c.sgn[:], ge[:], 2.0, -1.0, ALU.mult, ALU.add), reads=[ge], writes=[c.sgn])
    S.op("dve", lambda e: e.scalar_tensor_tensor(pf[:], ge[:], -16.0, pf[:], ALU.mult, ALU.add), reads=[ge, pf], writes=[pf])
    S.op("act", lambda e: e.activation(c.invf[:], pf[:], AF.Exp, scale=float(-2.0 * np.log(500000.0) / 32.0)), reads=[pf], writes=[c.invf])
    c.pf = pf
    c.one = S.sbuf("c_one", [128, 1], F32, st)
    S.op("pool", lambda e: e.memset(c.one[:], 1.0), writes=[c.one])
    c.zrow = S.sbuf("c_zrow", [1, 512], BF16, st)
    S.op("pool", lambda e: e.memset(c.zrow[:], 0.0), writes=[c.zrow])
    psw = S.sbuf("c_pswf", [128, 32], F32, st)
    c.pswap = S.sbuf("c_pswap", [128, 32], BF16, st)
    S.op("pool", lambda e: e.memset(psw[:], 0.0), writes=[psw])
    S.op("pool", lambda e: e.affine_select(psw[:, 0:16], psw[:, 0:16], [[-1, 16]], ALU.not_equal, 1.0, base=-16, channel_multiplier=1), reads=[psw], writes=[psw])
    S.op("pool", lambda e: e.affine_select(psw[:, 16:32], psw[:, 16:32], [[-1, 16]], ALU.not_equal, 1.0, base=0, channel_multiplier=1), reads=[psw], writes=[psw])
    S.op("dve", lambda e: e.tensor_copy(c.pswap[:], psw[:]), reads=[psw], writes=[c.pswap])
    c.anw = S.sbuf("c_anw", [128, KC], F32, st)
    S.dma("sp", c.anw[:], P.anw.ap, c.anw, P.anw)
    c.convw = S.sbuf("c_convw", [128, 24, 4], F32, st)
    S.dma("sp", c.convw[:], P.convw.ap, c.convw, P.convw)


def build(T, dbg=False, phases=("a0", "a1", "nsa", "gdn", "tail"), own_div=2):
    nc = bass.Bass("TRN2", target_bir_lowering=False)
    P = Prog()
    P.T, P.NT = T, T // 128
    st = ExitStack()
    with st:
        S = Sched(nc, st)
        sk = "ExternalOutput" if dbg else "Internal"
        P.x = S.dram("x", [T, D], F32, kind="ExternalInput")
        P.pos = S.dram("pos", [T], I32, kind="ExternalInput")
        P.anw = S.dram("anw", [128, KC], F32, kind="ExternalInput")
        P.w_in = S.dram("w_in", [D, 6696], F32, kind="ExternalInput")
        P.convw = S.dram("convw", [128, 24, 4], F32, kind="ExternalInput")
        P.cmp_wk = S.dram("cmp_wk", [32, 128, 128], F32, kind="ExternalInput")
        P.cmp_wv = S.dram("cmp_wv", [32, 128, 128], F32, kind="ExternalInput")
        P.cmp_pe = S.dram("cmp_pe", [128, 2, 32], F32, kind="ExternalInput")
        P.otm = S.dram("otm", [T, D], BF16, kind=sk)
        NO = P.NO = (T // 128) // own_div
        P.own_idx = S.dram("own_idx", [128, NO], I32, kind="ExternalInput")
        P.w_out = S.dram("w_out", [D, D], F32, kind="ExternalInput")
        P.fnw = S.dram("fnw", [D], F32, kind="ExternalInput")
        P.wr = S.dram("wr", [D, 72], F32, kind="ExternalInput")
        P.br = S.dram("br", [72], F32, kind="ExternalInput")
        P.wg = S.dram("wg", [64, D, 512], F32, kind="ExternalInput")
        P.wu = S.dram("wu", [64, D, 512], F32, kind="ExternalInput")
        P.wd = S.dram("wd", [64, 512, D], F32, kind="ExternalInput")
        P.moe_w = P.wg
        P.finw = S.dram("finw", [D], F32, kind="ExternalInput")
        P.h1 = S.dram("h1", [NO * 128, D], F32, kind=sk)
        P.xdisp = S.dram("xdisp", [64 * CAP, D], BF16, kind=sk)
        P.yall = S.dram("yall", [64 * CAP, D], F32, kind=sk)
        P.out = S.dram("out", [NO * 128, D], F32, kind="ExternalOutput")
        P.gpar = S.dram("gpar", [16], F32, kind="ExternalInput")
        P.gnw = S.dram("gnw", [128], F32, kind="ExternalInput")
        P.own_chunks = list(range(T // 512))
        P.xT = S.dram("xT", [128, KC, T], BF16, kind=sk)
        P.qT = S.dram("qT", [8, 128, T], BF16, kind=sk)
        P.kT = S.dram("kT", [6, 128, T], BF16, kind=sk)
        P.vcT = S.dram("vcT", [2, 128, T], BF16, kind=sk)
        P.vs = S.dram("vs", [T, 256], BF16, kind=sk)
        P.vw = S.dram("vw", [T, 256], BF16, kind=sk)
        P.gT = S.dram("gT", [24, 128, T], BF16, kind=sk)
        P.zs = S.dram("zs", [T, 1024], BF16, kind=sk)
        P.gate = S.dram("gate", [T, 24], F32, kind=sk)
        P.bd = S.dram("bd", [T, 16], F32, kind=sk)
        ps = [S.psum("ps%d" % i, [128, 512], F32, st) for i in range(8)]
        c = build_consts(S, P, st)
        more_consts(S, P, c, st)
        S.barrier()
        if "a0" in phases:
            with nc.named_scope("phase_a0"):
                phase_a0(S, P, c, ps)
            S.barrier()
        if "a1" in phases:
            with nc.named_scope("phase_a1"):
                phase_a1(S, P, c, ps)
            S.barrier()
        if "nsa" in phases:
            with nc.named_scope("phase_nsa"):
                phase_nsa(S, P, c, ps)
            S.barrier()
        if "gdn" in phases:
            with nc.named_scope("phase_gdn"):
                phase_gdn(S, P, c, ps)
            S.barrier()
        if "tail" in phases:
            R = Prog()
            R.w = S.sbuf("r_w", [128, NO, 2], F32, st)
            R.loc = S.sbuf("r_loc", [128, NO, 2], I32, st)
            with nc.named_scope("phase_tail1"):
                phase_tail1(S, P, c, ps, R)
            S.barrier()
            with nc.named_scope("phase_tail2"):
                phase_tail2(S, P, c, ps)
            S.barrier()
            with nc.named_scope("phase_tail3"):
                phase_tail3(S, P, c, ps, R)
            S.barrier()
        outs = [P.out, P.otm, P.xT, P.qT, P.kT, P.vcT, P.vs, P.vw, P.gT, P.zs, P.gate, P.bd]
        for e in ("sp", "pool", "act", "dve", "pe"):
            S.wait_all(e, outs)
        print("instructions:", S.n_ins, "dsems:", S.n_dsem)
    return nc


def phase_nsa(S, P, c, ps):
    T, NT = P.T, P.NT
    NCMP = T // 16 - 1
    NNT = (NCMP + 127) // 128
    NSLC = T // 64
    SC = float(HD ** -0.5)
    with ExitStack() as st:
        kT = [S.sbuf("n_kT%d" % i, [128, T], BF16, st) for i in range(6)]
        for i in range(6):
            S.dma("sp", kT[i][:], P.kT.ap[i], kT[i], P.kT)
        V1 = {}
        for bi, (nm, src) in enumerate((("s", P.vs), ("w", P.vw))):
            for g in range(2):
                v = S.sbuf("n_v%s%d" % (nm, g), [128, NT, 129], BF16, st)
                S.op("pool", lambda e: e.memset(v[:, :, 128:129], 1.0), writes=[v])
                S.dma("sp", v[:, :, 0:128], src.ap[:, g * 128:(g + 1) * 128].rearrange("(n p) d -> p n d", p=128), v, src)
                V1[(nm, g)] = v
        caus = S.sbuf("n_caus", [128, 128], BF16, st)
        caus2 = S.sbuf("n_caus2", [128, 128], BF16, st)
        cb_pre = [S.sbuf("n_cb%d" % nt, [128, T], BF16, st) for nt in range(NNT)]
        esel = S.sbuf("n_esel", [64, T], BF16, st)
        cbv = S.sbuf("n_cbv", [128, 2], F32, st)
        kcT = [S.sbuf("n_kcT%d" % g, [128, NNT * 128], BF16, st) for g in range(2)]
        rhsC = [S.sbuf("n_rhsC%d" % g, [128, NNT, 193], BF16, st) for g in range(2)]
        st2 = ExitStack()
        zf = S.sbuf("n_zf", [128, 128], F32, st2)
        S.op("pool", lambda e: e.memset(zf[:], 0.0), writes=[zf])
        S.op("pool", lambda e: e.affine_select(caus[:], zf[:], [[1, 128]], ALU.is_ge, NEG, base=0, channel_multiplier=-1), reads=[zf], writes=[caus])
        S.op("pool", lambda e: e.affine_select(caus2[:], zf[:], [[-1, 128]], ALU.is_gt, NEG, base=0, channel_multiplier=1), reads=[zf], writes=[caus2])
        zT = S.sbuf("n_zT", [128, T], BF16, st2)
        S.op("pool", lambda e: e.memset(zT[:], 0.0), writes=[zT])
        cbias = []
        for nt in range(NNT):
            cb = cb_pre[nt]
            S.op("pool", lambda e: e.affine_select(cb[:], zT[:], [[1, T]], ALU.is_ge, NEG, base=-(16 * nt * 128) - 31, channel_multiplier=-16),
                 reads=[zT], writes=[cb])
            cbias.append(cb)
        oneT = S.sbuf("n_oneT", [64, T], BF16, st2)
        S.op("pool", lambda e: e.memset(oneT[:], 1.0), writes=[oneT])
        S.op("pool", lambda e: e.affine_select(esel[:], oneT[:], [[1, T]], ALU.is_ge, 0.0, base=0, channel_multiplier=-64), reads=[oneT], writes=[esel])
        S.op("pool", lambda e: e.affine_select(esel[:], esel[:], [[-1, T]], ALU.is_ge, 0.0, base=63, channel_multiplier=64), reads=[esel], writes=[esel])
        wkf = S.sbuf("n_wkf", [128, 32, 128], F32, st2)
        wkb = [S.sbuf("n_wkb%d" % i, [128, 32, 128], BF16, st2) for i in range(2)]
        pef = S.sbuf("n_pef", [128, 2, 32], F32, st2)
        peb = S.sbuf("n_peb", [128, 2, 32], BF16, st2)
        S.dma("sp", pef[:], P.cmp_pe.ap, pef, P.cmp_pe)
        S.op("dve", lambda e: e.tensor_copy(peb[:], pef[:]), reads=[pef], writes=[peb])
        for i, src in enumerate((P.cmp_wk, P.cmp_wv)):
            S.dma("sp", wkf[:], src.ap.rearrange("l d e -> d l e"), wkf, src)
            S.op("dve", lambda e: e.tensor_copy(wkb[i][:], wkf[:]), reads=[wkf], writes=[wkb[i]])
        for i in range(2):
            pb = ps[6]
            for l in range(32):
                S.op("pe", lambda e: e.matmul(pb[:, 0:1], wkb[i][:, l, :], peb[:, i, l:l + 1], start=(l == 0), stop=(l == 31)),
                     reads=[wkb[i], peb], writes=[pb])
            S.op("act", lambda e: e.copy(cbv[:, i:i + 1], pb[:, 0:1]), reads=[pb], writes=[cbv])
        vcs = S.sbuf("n_vcs", [128, T], BF16, st2)
        vcTs = S.sbuf("n_vcTs", [128, NNT * 128], BF16, st2)
        for g in range(2):
            S.op("pool", lambda e: e.memset(kcT[g][:], 0.0), writes=[kcT[g]])
            S.op("pool", lambda e: e.memset(rhsC[g][:], 0.0), writes=[rhsC[g]])
            S.op("pool", lambda e: e.memset(rhsC[g][:, :, 128:129], 1.0), writes=[rhsC[g]])
            for nt in range(NNT):
                a = rhsC[g][:, nt, 129:129 + NSLC]
                S.op("pool", lambda e: e.memset(a, 1.0), writes=[rhsC[g]])
                S.op("pool", lambda e: e.affine_select(a, a, [[-4, NSLC]], ALU.is_ge, 0.0, base=nt * 128 + 1, channel_multiplier=1), reads=[rhsC[g]], writes=[rhsC[g]])
                S.op("pool", lambda e: e.affine_select(a, a, [[4, NSLC]], ALU.is_ge, 0.0, base=3 - nt * 128, channel_multiplier=-1), reads=[rhsC[g]], writes=[rhsC[g]])
        for g in range(2):
            S.dma("sp", vcs[:], P.vcT.ap[g], vcs, P.vcT)
            for i, (src, dst) in enumerate(((kT[0 * 2 + g], kcT[g]), (vcs, vcTs))):
                if i == 1:
                    S.op("pool", lambda e: e.memset(vcTs[:], 0.0), writes=[vcTs])
                for n0 in range(0, NCMP, 512):
                    nn = min(512, NCMP - n0)
                    pb = ps[6]
                    for l in range(32):
                        S.op("pe", lambda e: e.matmul(pb[:, 0:nn], wkb[i][:, l, :], src[:, 16 * n0 + l:16 * (n0 + nn - 1) + l + 1:16],
                                                      start=(l == 0), stop=(l == 31)),
                             reads=[wkb[i], src], writes=[pb])
                    S.op("act", lambda e: e.activation(dst[:, n0:n0 + nn], pb[:, 0:nn], AF.Identity, bias=cbv[:, i:i + 1]),
                         reads=[pb, cbv], writes=[dst])
            for nt in range(NNT):
                pb = ps[7]
                pv = pb.ap.bitcast(BF16)
                S.op("pe", lambda e: e.transpose(pv[:, 0:128], vcTs[:, nt * 128:(nt + 1) * 128], c.ident[:]), reads=[vcTs, c.ident], writes=[pb])
                S.op("act", lambda e: e.copy(rhsC[g][:, nt, 0:128], pv[:, 0:128]), reads=[pb], writes=[rhsC[g]])
            if NCMP % 128:
                pass
        S.barrier()
        st2.close()
        mbT = [S.sbuf("n_mbT%d" % g, [64, T], BF16, st) for g in range(2)]
        qs = [S.sbuf("n_q%d" % i, [128, 512], BF16, st) for i in range(2)]
        pT = [S.sbuf("n_pT%d" % i, [128, 512], BF16, st) for i in range(2)]
        oa = S.sbuf("n_oa", [128, 4, 1024], F32, st)
        oab = S.sbuf("n_oab", [128, 4, 1024], BF16, st)
        gt = S.sbuf("n_gt", [128, 4, 24], F32, st)
        imp = S.sbuf("n_imp", [128, 4, 64], F32, st)
        rl = S.sbuf("n_rl", [128, 8], F32, st)
        rls = [S.sbuf("n_rls%d" % i, [128, 2, 2], F32, st) for i in range(2)]
        ptmp = [S.sbuf("n_ptmp%d" % i, [128, 2, 128], F32, st) for i in range(2)]
        hbc = [0]
        tk = S.sbuf("n_tk", [128, 6, 64], F32, st)
        m8 = S.sbuf("n_m8", [128, 16], F32, st)
        mb = S.sbuf("n_mb", [128, 64], BF16, st)
        Dm = S.sbuf("n_Dm", [128, 64], F32, st)
        e0 = S.sbuf("n_e0", [128, 64], F32, st)
        di = S.sbuf("n_di", [128, 64], I32, st)
        S.op("pool", lambda e: e.iota(di[:], [[1, 64]], base=0, channel_multiplier=0), writes=[di])
        S.op("dve", lambda e: e.tensor_copy(Dm[:], di[:]), reads=[di], writes=[Dm])
        S.op("dve", lambda e: e.tensor_scalar(e0[:], Dm[:], 0.0, None, ALU.is_equal), reads=[Dm], writes=[e0])
        S.op("dve", lambda e: e.tensor_scalar(Dm[64:128, :], Dm[64:128, :], -1.0, None, ALU.add), reads=[Dm], writes=[Dm])
        cnt = [0]
        BIG = 1.0e4

        def run_items(items):
            n = len(items)
            for i in range(n + 1):
                if i < n:
                    items[i]["A"](ps[i % 2])
                if i >= 1:
                    it = items[i - 1]
                    it["B"](ps[(i - 1) % 2], pT[(i - 1) % 2])
                    it["C"](pT[(i - 1) % 2])
                    if it.get("post"):
                        it["post"]()

        qbuf = {}

        def load_q(h, tsl):
            q = qs[h % 2]
            S.dma("sp", q[:], P.qT.ap[h][:, tsl], q, P.qT)
            qbuf[h] = q

        for ch in P.own_chunks:
            q0 = ch * 4
            tsl = slice(ch * 512, (ch + 1) * 512)
            S.dma("sp", gt[:], P.gate.ap[tsl, :].rearrange("(n p) c -> p n c", p=128), gt, P.gate)
            S.op("act", lambda e: e.activation(gt[:], gt[:], AF.Sigmoid), reads=[gt], writes=[gt])
            accs = [(ps[2 + qi], 0) for qi in range(4)]
            for g in range(2):
                S.op("pool", lambda e: e.memset(imp[:], 0.0), writes=[imp])
                items = []
                for r in range(4):
                    h = g * 4 + r
                    hbc[0] += 1
                    hb = hbc[0]
                    for nt in range(NNT):
                        def A(sp, h=h, nt=nt, r=r):
                            if nt == 0:
                                if r == 0:
                                    load_q(h, tsl)
                                if r < 3:
                                    load_q(h + 1, tsl)
                            q = qbuf[h]
                            S.op("pe", lambda e: e.matmul(sp[:, :], kcT[g][:, nt * 128:(nt + 1) * 128], q[:], start=True, stop=False),
                                 reads=[kcT[g], q], writes=[sp])
                            S.op("pe", lambda e: e.matmul(sp[:, :], c.ident[:], cbias[nt][:, tsl], start=False, stop=True),
                                 reads=[c.ident, cbias[nt]], writes=[sp])

                        def B(sp, p):
                            S.op("act", lambda e: e.activation(p[:, :], sp[:, :], AF.Exp, scale=SC), reads=[sp], writes=[p])

                        def C(p, nt=nt, hb=hb):
                            bset = [ps[2 + 2 * (hb % 2)], ps[3 + 2 * (hb % 2)]]
                            if nt == 0:
                                for ab in bset:
                                    S.op("pe", lambda e: e.matmul(ab[:, 0:386], c.zrow[0:1, 0:128], c.zrow[0:1, 0:386], start=True, stop=False),
                                         reads=[c.zrow], writes=[ab])
                            for qi in range(4):
                                ab = bset[qi // 2]
                                off = (qi % 2) * 193
                                S.op("pe", lambda e: e.matmul(ab[:, off:off + 193], p[:, qi * 128:(qi + 1) * 128], rhsC[g][:, nt, :],
                                                              start=False, stop=(nt == NNT - 1 and qi % 2 == 1)),
                                     reads=[p, rhsC[g]], writes=[ab])

                        def post(h=h, hb=hb):
                            bset = [ps[2 + 2 * (hb % 2)], ps[3 + 2 * (hb % 2)]]
                            for bi, ab in enumerate(bset):
                                r_ = rls[bi]
                                v = ab[:, 0:386].rearrange("p (j w) -> p j w", j=2)
                                qsl = slice(bi * 2, bi * 2 + 2)
                                S.op("dve", lambda e: e.tensor_scalar(r_[:, :, 0:1], v[:, :, 128:129], 1e-30, None, ALU.max), reads=[ab], writes=[r_])
                                S.op("dve", lambda e: e.reciprocal(r_[:, :, 0:1], r_[:, :, 0:1]), reads=[r_], writes=[r_])
                                S.op("dve", lambda e: e.tensor_tensor(r_[:, :, 1:2], r_[:, :, 0:1], gt[:, qsl, h * 3:h * 3 + 1], ALU.mult), reads=[r_, gt], writes=[r_])
                                t_ = ptmp[bi]
                                S.op("dve", lambda e: e.tensor_tensor(t_[:, :, 0:64], v[:, :, 129:193], r_[:, :, 0:1].to_broadcast([128, 2, 64]), ALU.mult), reads=[ab, r_], writes=[t_])
                                S.op("pool", lambda e: e.tensor_tensor(imp[:, qsl, :], imp[:, qsl, :], t_[:, :, 0:64], ALU.add), reads=[imp, t_], writes=[imp])
                                S.op("dve", lambda e: e.tensor_tensor(oa[:, qsl, h * 128:(h + 1) * 128], v[:, :, 0:128], r_[:, :, 1:2].to_broadcast([128, 2, 128]), ALU.mult),
                                     reads=[ab, r_], writes=[oa])
                        items.append({"A": A, "B": B, "C": C, "post": post if nt == NNT - 1 else None})
                run_items(items)
                for qi in range(4):
                    qt = q0 + qi
                    if NSLC <= 16:
                        S.op("pool", lambda e: e.memset(mb[:], 0.0), writes=[mb])
                    else:
                        a, b_, ip, wk_, mk = tk[:, 0, :], tk[:, 1, :], tk[:, 2, :], tk[:, 3, :], tk[:, 4, :]
                        S.op("dve", lambda e: e.tensor_scalar(a, Dm[:], float(2 * qt - 1), None, ALU.is_ge), reads=[Dm], writes=[tk])
                        S.op("dve", lambda e: e.tensor_tensor(a, a, e0[:], ALU.max), reads=[tk, e0], writes=[tk])
                        S.op("dve", lambda e: e.scalar_tensor_tensor(ip, a, BIG, imp[:, qi, :], ALU.mult, ALU.add), reads=[tk, imp], writes=[tk])
                        S.op("dve", lambda e: e.tensor_scalar(b_, Dm[:], float(2 * qt), None, ALU.is_gt), reads=[Dm], writes=[tk])
                        S.op("dve", lambda e: e.scalar_tensor_tensor(ip, b_, -3.0 * BIG, ip, ALU.mult, ALU.add), reads=[tk], writes=[tk])
                        S.op("dve", lambda e: e.max(m8[:, 0:8], ip), reads=[tk], writes=[m8])
                        S.op("dve", lambda e: e.match_replace(wk_, m8[:, 0:8], ip, -1.0e9), reads=[tk, m8], writes=[tk])
                        S.op("dve", lambda e: e.max(m8[:, 8:16], wk_), reads=[tk], writes=[m8])
                        S.op("dve", lambda e: e.tensor_scalar(mk, ip, m8[:, 15:16], None, ALU.is_ge), reads=[tk, m8], writes=[tk])
                        S.op("dve", lambda e: e.tensor_scalar(mb[:], mk, -1.0, -NEG, ALU.add, ALU.mult), reads=[tk], writes=[mb])
                    pb = ps[6]
                    pv = pb.ap.bitcast(BF16)
                    S.op("pe", lambda e: e.transpose(pv[0:64, 0:128], mb[:], c.ident[:]), reads=[mb, c.ident], writes=[pb])
                    S.op("act", lambda e: e.copy(mbT[g][:, qt * 128:(qt + 1) * 128], pv[0:64, 0:128]), reads=[pb], writes=[mbT[g]])
                items = []
                for r in range(4):
                    h = g * 4 + r
                    for br, nm in ((1, "s"), (2, "w")):
                        kt_lo = 0 if br == 1 else max(0, q0 - 4)
                        kts = list(range(kt_lo, q0 + 4))
                        K_ = kT[br * 2 + g]
                        V_ = V1[(nm, g)]
                        hbc[0] += 1
                        hb = hbc[0]
                        for kt in kts:
                            qlo_t = max(kt, q0)
                            qhi_t = q0 + 3 if br == 1 else min(q0 + 3, kt + 4)
                            c_lo = (qlo_t - q0) * 128
                            c_hi = (qhi_t - q0 + 1) * 128

                            def A(sp, h=h, r=r, br=br, kt=kt, c_lo=c_lo, c_hi=c_hi, K_=K_, first=(br == 1 and kt == kts[0])):
                                if first:
                                    if r == 0:
                                        load_q(h, tsl)
                                    if r < 3:
                                        load_q(h + 1, tsl)
                                q = qbuf[h]
                                mm = [(slice(c_lo, c_hi), K_[:, kt * 128:(kt + 1) * 128], q[:, c_lo:c_hi], [K_, q])]
                                if kt >= q0:
                                    d0 = (kt - q0) * 128
                                    mm.append((slice(d0, d0 + 128), c.ident[:], caus[:], [c.ident, caus]))
                                if br == 2 and q0 <= kt + 4 <= q0 + 3:
                                    d0 = (kt + 4 - q0) * 128
                                    mm.append((slice(d0, d0 + 128), c.ident[:], caus2[:], [c.ident, caus2]))
                                if br == 1:
                                    mm.append((slice(c_lo, c_hi), esel[:, kt * 128:(kt + 1) * 128], mbT[g][:, ch * 512 + c_lo:ch * 512 + c_hi], [esel, mbT[g]]))
                                for mi, (osl, l_, r_, rd) in enumerate(mm):
                                    S.op("pe", lambda e: e.matmul(sp[:, osl], l_, r_, start=(mi == 0), stop=(mi == len(mm) - 1)), reads=rd, writes=[sp])

                            def B(sp, p, c_lo=c_lo, c_hi=c_hi):
                                S.op("act", lambda e: e.activation(p[:, c_lo:c_hi], sp[:, c_lo:c_hi], AF.Exp, scale=SC), reads=[sp], writes=[p])

                            def C(p, kt=kt, qlo_t=qlo_t, qhi_t=qhi_t, V_=V_, hb=hb, firstkt=(kt == kts[0])):
                                bset = [ps[2 + 2 * (hb % 2)], ps[3 + 2 * (hb % 2)]]
                                if firstkt:
                                    for ab in bset:
                                        S.op("pe", lambda e: e.matmul(ab[:, 0:258], c.zrow[0:1, 0:128], c.zrow[0:1, 0:258], start=True, stop=False),
                                             reads=[c.zrow], writes=[ab])
                                for qt in range(qlo_t, qhi_t + 1):
                                    qi = qt - q0
                                    ab = bset[qi // 2]
                                    off = (qi % 2) * 129
                                    S.op("pe", lambda e: e.matmul(ab[:, off:off + 129], p[:, qi * 128:(qi + 1) * 128], V_[:, kt, :],
                                                                  start=False, stop=(kt == qt and qi % 2 == 1)),
                                         reads=[p, V_], writes=[ab])

                            def post(h=h, br=br, hb=hb):
                                bset = [ps[2 + 2 * (hb % 2)], ps[3 + 2 * (hb % 2)]]
                                col = h * 3 + br
                                for bi, ab in enumerate(bset):
                                    r_ = rls[bi]
                                    v = ab[:, 0:258].rearrange("p (j w) -> p j w", j=2)
                                    qsl = slice(bi * 2, bi * 2 + 2)
                                    S.op("dve", lambda e: e.tensor_scalar(r_[:, :, 0:1], v[:, :, 128:129], 1e-30, None, ALU.max), reads=[ab], writes=[r_])
                                    S.op("dve", lambda e: e.reciprocal(r_[:, :, 0:1], r_[:, :, 0:1]), reads=[r_], writes=[r_])
                                    S.op("dve", lambda e: e.tensor_tensor(r_[:, :, 1:2], r_[:, :, 0:1], gt[:, qsl, col:col + 1], ALU.mult), reads=[r_, gt], writes=[r_])
                                    t_ = ptmp[bi]
                                    S.op("dve", lambda e: e.tensor_tensor(t_[:, :, :], v[:, :, 0:128], r_[:, :, 1:2].to_broadcast([128, 2, 128]), ALU.mult), reads=[ab, r_], writes=[t_])
                                    S.op("pool", lambda e: e.tensor_tensor(oa[:, qsl, h * 128:(h + 1) * 128], oa[:, qsl, h * 128:(h + 1) * 128], t_[:, :, :], ALU.add),
                                         reads=[oa, t_], writes=[oa])
                            items.append({"A": A, "B": B, "C": C, "post": post if kt == kts[-1] else None})
                run_items(items)
            S.op("act", lambda e: e.copy(oab[:], oa[:]), reads=[oa], writes=[oab])
            S.dma("pool", P.otm.ap[tsl, 0:1024].rearrange("(q p) c -> p q c", p=128), oab[:], P.otm, oab)


def phase_gdn(S, P, c, ps):
    T, NT = P.T, P.NT
    SCQ = float(HD ** -0.5)
    with ExitStack() as st:
        def mk(name):
            return S.sbuf(name, [128, 128], F32, st)
        ones = mk("g_ones"); Bd = mk("g_Bd"); LtriT = mk("g_LtriT"); RtriT = mk("g_RtriT"); U = mk("g_U"); LsT = mk("g_LsT")
        C0 = mk("g_C0"); C1 = mk("g_C1")
        S.op("pool", lambda e: e.memset(ones[:], 1.0), writes=[ones])
        S.op("pool", lambda e: e.memset(Bd[:], 1.0), writes=[Bd])
        S.op("pool", lambda e: e.memset(Bd[0:64, 64:128], 0.0), writes=[Bd])
        S.op("pool", lambda e: e.memset(Bd[64:128, 0:64], 0.0), writes=[Bd])
        S.op("pool", lambda e: e.memset(C0[:], 0.0), writes=[C0])
        S.op("pool", lambda e: e.memset(C0[0:64, :], 1.0), writes=[C0])
        S.op("pool", lambda e: e.memset(C1[:], 0.0), writes=[C1])
        S.op("pool", lambda e: e.memset(C1[64:128, :], 1.0), writes=[C1])
        S.op("pool", lambda e: e.affine_select(LtriT[:], Bd[:], [[1, 128]], ALU.is_ge, 0.0, base=0, channel_multiplier=-1), reads=[Bd], writes=[LtriT])
        S.op("pool", lambda e: e.affine_select(RtriT[:], Bd[:], [[-1, 128]], ALU.is_gt, 0.0, base=0, channel_multiplier=1), reads=[Bd], writes=[RtriT])
        S.op("pool", lambda e: e.affine_select(U[:], ones[:], [[-1, 128]], ALU.is_gt, 0.0, base=0, channel_multiplier=1), reads=[ones], writes=[U])
        S.op("pool", lambda e: e.affine_select(LsT[:], Bd[:], [[1, 128]], ALU.is_gt, 0.0, base=0, channel_multiplier=-1), reads=[Bd], writes=[LsT])
        nR = mk("g_nR"); nLs = mk("g_nLs")
        S.op("dve", lambda e: e.tensor_scalar(nR[:], RtriT[:], -1.0, None, ALU.mult), reads=[RtriT], writes=[nR])
        S.op("dve", lambda e: e.tensor_scalar(nLs[:], LsT[:], -1.0, None, ALU.mult), reads=[LsT], writes=[nLs])
        par = S.sbuf("g_par", [128, 16], F32, st)
        nw = S.sbuf("g_nw", [128, 128], F32, st)
        S.dma("sp", par[:], P.gpar.ap.partition_broadcast(128), par, P.gpar)
        S.dma("sp", nw[:], P.gnw.ap.partition_broadcast(128), nw, P.gnw)
        nexpA = S.sbuf("g_nexpA", [128, 8], F32, st)
        S.op("act", lambda e: e.activation(nexpA[:], par[:, 0:8], AF.Exp), reads=[par], writes=[nexpA])
        S.op("dve", lambda e: e.tensor_scalar(nexpA[:], nexpA[:], -1.0, None, ALU.mult), reads=[nexpA], writes=[nexpA])
        Sf = S.sbuf("g_Sf", [128, 8, 128], F32, st)
        Sb = S.sbuf("g_Sb", [128, 8, 128], BF16, st)
        S.op("pool", lambda e: e.memset(Sf[:], 0.0), writes=[Sf])
        S.op("pool", lambda e: e.memset(Sb[:], 0.0), writes=[Sb])
        gin = [S.sbuf("g_in%d" % i, [128, 24, 128], BF16, st) for i in range(2)]
        bdr = [S.sbuf("g_bd%d" % i, [128, 16], F32, st) for i in range(2)]
        tm = S.sbuf("g_tm", [128, 24, 128], BF16, st)
        junk = S.sbuf("g_junk", [128, 128], BF16, st)
        sq16 = S.sbuf("g_sq16", [128, 16, 128], F32, st)
        bsc = S.sbuf("g_bsc", [128, 4, 8], F32, st)
        E = S.sbuf("g_E", [128, 8, 2, 128], F32, st)
        tmpf = S.sbuf("g_tmpf", [128, 4, 128], F32, st)
        ssq = S.sbuf("g_ssq", [128, 16], F32, st)
        rn = S.sbuf("g_rn", [128, 16], F32, st)
        sc = S.sbuf("g_sc", [128, 8, 8], F32, st)
        gv = S.sbuf("g_gv", [128, 8], F32, st)
        egsL = [S.sbuf("g_egs%d" % i, [128, 32], F32, st) for i in range(2)]
        ssq2 = S.sbuf("g_ssq2", [128, 16], F32, st)
        rn2 = S.sbuf("g_rn2", [128, 16], F32, st)
        sq8 = S.sbuf("g_sq8", [128, 16, 128], F32, st)
        khat = S.sbuf("g_khat", [128, 8, 128], BF16, st)
        kb = S.sbuf("g_kb", [128, 8, 128], BF16, st)
        kbg = S.sbuf("g_kbg", [128, 8, 128], BF16, st)
        kdecL = [S.sbuf("g_kdec%d" % i, [128, 8, 128], BF16, st) for i in range(2)]
        qs_ = S.sbuf("g_qs", [128, 8, 128], BF16, st)
        qd = S.sbuf("g_qd", [128, 8, 128], BF16, st)
        vb = S.sbuf("g_vb", [128, 8, 128], BF16, st)
        khT = S.sbuf("g_khT", [128, 8, 128], BF16, st)
        kbT = S.sbuf("g_kbT", [128, 8, 128], BF16, st)
        qsT = S.sbuf("g_qsT", [128, 8, 128], BF16, st)
        qdTL = [S.sbuf("g_qdT%d" % i, [128, 8, 128], BF16, st) for i in range(2)]
        GU = [S.sbuf("g_GU%d" % i, [128, 128], F32, st) for i in range(2)]
        Pm = [S.sbuf("g_P%d" % i, [128, 8, 128], BF16, st) for i in range(2)]
        PTm = [S.sbuf("g_PT%d" % i, [128, 8, 128], BF16, st) for i in range(2)]
        Xm = [S.sbuf("g_X%d" % i, [128, 8, 128], BF16, st) for i in range(2)]
        atTL = [S.sbuf("g_atT%d" % i, [128, 8, 128], BF16, st) for i in range(2)]
        uL = [S.sbuf("g_u%d" % i, [128, 8, 128], F32, st) for i in range(2)]
        wTL = [S.sbuf("g_wT%d" % i, [128, 8, 128], BF16, st) for i in range(2)]
        vn = S.sbuf("g_vn", [128, 8, 128], BF16, st)
        o = S.sbuf("g_o", [128, 8, 128], F32, st)
        ob = S.sbuf("g_ob", [128, 1024], BF16, st)
        zt = S.sbuf("g_zt", [128, 1024], BF16, st)
        oTs = [S.sbuf("g_oT%d" % i, [128, 512], BF16, st) for i in range(2)]
        obc = S.sbuf("g_obc", [128, 4, 1024], BF16, st)
        rot = [0]

        def bank():
            rot[0] += 1
            return ps[rot[0] % 8]

        def load(i, b):
            S.dma("sp", gin[b][:], P.gT.ap[:, :, i * 128:(i + 1) * 128].rearrange("b p t -> p b t"), gin[b], P.gT)
            S.dma("sp", bdr[b][:], P.bd.ap[i * 128:(i + 1) * 128, :], bdr[b], P.bd)

        def transpose_group(dst, src, n, evac):
            for j0 in range(0, n, 4):
                pb = bank()
                pv = pb.ap.bitcast(BF16)
                for j in range(j0, min(n, j0 + 4)):
                    S.op("pe", lambda e: e.transpose(pv[:, (j - j0) * 128:(j - j0 + 1) * 128], src[:, j, :], c.ident[:]), reads=[src, c.ident], writes=[pb])
                nn = min(n, j0 + 4) - j0
                if evac == "act":
                    S.op("act", lambda e: e.copy(dst[:, j0:j0 + nn, :], pv[:, 0:nn * 128].rearrange("p (j t) -> p j t", j=nn)), reads=[pb], writes=[dst])
                else:
                    S.op("dve", lambda e: e.tensor_copy(dst[:, j0:j0 + nn, :], pv[:, 0:nn * 128].rearrange("p (j t) -> p j t", j=nn)), reads=[pb], writes=[dst])

        def prep_gen(i):
            b = i % 2
            own = (i // 4) in P.own_chunks
            egs = egsL[i % 2]; kdec = kdecL[i % 2]; qdT = qdTL[i % 2]; atT = atTL[i % 2]; u = uL[i % 2]; wT = wTL[i % 2]
            load(i, b)
            G = gin[b]
            transpose_group(tm, G, 24, "act")
            yield
            bd_ = bdr[b]
            S.op("act", lambda e: e.activation(sc[:, :, 0], bd_[:, 0:8], AF.Sigmoid), reads=[bd_], writes=[sc])
            S.op("dve", lambda e: e.tensor_tensor(gv[:], bd_[:, 8:16], par[:, 8:16], ALU.add), reads=[bd_, par], writes=[gv])
            S.op("act", lambda e: e.activation(gv[:], gv[:], AF.Exp), reads=[gv], writes=[gv])
            S.op("act", lambda e: e.activation(gv[:], gv[:], AF.Ln, bias=c.one[:, 0:1]), reads=[gv, c.one], writes=[gv])
            S.op("dve", lambda e: e.tensor_tensor(gv[:], gv[:], nexpA[:], ALU.mult), reads=[gv, nexpA], writes=[gv])
            pg = bank()
            for j, M_ in enumerate((LtriT, RtriT, C0, C1)):
                S.op("pe", lambda e: e.matmul(pg[:, j * 8:(j + 1) * 8], M_[:], gv[:], start=True, stop=True), reads=[M_, gv], writes=[pg])
            S.op("act", lambda e: e.activation(egs[:], pg[:, 0:32], AF.Exp), reads=[pg], writes=[egs])
            yield
            S.op("act", lambda e: e.activation(sq16[:], tm[:, 0:16, :], AF.Square), reads=[tm], writes=[sq16])
            S.op("dve", lambda e: e.tensor_reduce(ssq[:, 0:16], sq16[:], AX.X, ALU.add), reads=[sq16], writes=[ssq])
            S.op("act", lambda e: e.activation(rn[:], ssq[:], AF.Sqrt, bias=c.eps[:, 0:1]), reads=[ssq, c.eps], writes=[rn])
            S.op("dve", lambda e: e.reciprocal(rn[:], rn[:]), reads=[rn], writes=[rn])
            def bc(ap8):
                return ap8.unsqueeze(2).to_broadcast([128, 8, 128])
            beta8 = sc[:, :, 0]
            S.op("dve", lambda e: e.tensor_scalar(bsc[:, 0, :], beta8, -1.0, None, ALU.mult), reads=[sc], writes=[bsc])
            S.op("dve", lambda e: e.tensor_tensor(bsc[:, 1, :], beta8, egs[:, 0:8], ALU.mult), reads=[sc, egs], writes=[bsc])
            S.op("dve", lambda e: e.tensor_scalar(bsc[:, 2, :], rn[:, 0:8], SCQ, None, ALU.mult), reads=[rn], writes=[bsc])
            S.op("dve", lambda e: e.tensor_tensor(khat[:], tm[:, 8:16, :], bc(rn[:, 8:16]), ALU.mult), reads=[tm, rn], writes=[khat])
            S.op("dve", lambda e: e.tensor_tensor(kb[:], khat[:], bc(bsc[:, 0, :]), ALU.mult), reads=[khat, bsc], writes=[kb])
            S.op("pool", lambda e: e.tensor_tensor(kbg[:], khat[:], bc(bsc[:, 1, :]), ALU.mult), reads=[khat, bsc], writes=[kbg])
            S.op("pool", lambda e: e.tensor_tensor(kdec[:], khat[:], bc(egs[:, 8:16]), ALU.mult), reads=[khat, egs], writes=[kdec])
            S.op("dve", lambda e: e.tensor_tensor(qs_[:], tm[:, 0:8, :], bc(bsc[:, 2, :]), ALU.mult), reads=[tm, bsc], writes=[qs_])
            S.op("dve", lambda e: e.tensor_tensor(qd[:], qs_[:], bc(egs[:, 0:8]), ALU.mult), reads=[qs_, egs], writes=[qd])
            S.op("pool", lambda e: e.tensor_tensor(vb[:], tm[:, 16:24, :], bc(beta8), ALU.mult), reads=[tm, sc], writes=[vb])
            transpose_group(khT, khat, 8, "act")
            yield
            transpose_group(kbT, kb, 8, "act")
            yield
            transpose_group(qsT, qs_, 8, "act")
            yield
            transpose_group(qdT, qd, 8, "act")
            yield
            for h in range(8):
                gu = GU[h % 2]
                S.op("dve", lambda e: e.tensor_scalar(gu[:], U[:], gv[:, h:h + 1], None, ALU.mult), reads=[U, gv], writes=[gu])
                pd = bank()
                S.op("pe", lambda e: e.matmul(pd[:, 0:128], LtriT[:], gu[:], start=True, stop=True), reads=[LtriT, gu], writes=[pd])
                S.op("pe", lambda e: e.matmul(pd[:, 128:256], gu[:], LtriT[:], start=True, stop=True), reads=[LtriT, gu], writes=[pd])
                S.op("act", lambda e: e.activation(E[:, h, :, :], pd[:, 0:256].rearrange("p (a t) -> p a t", a=2), AF.Exp), reads=[pd], writes=[E])
                if h % 2 == 1:
                    yield
            P0, PT0, X0 = Pm[0], PTm[0], Xm[0]
            for hg in range(2):
                hs = range(hg * 4, hg * 4 + 4)
                pb = bank()
                for h in hs:
                    S.op("pe", lambda e: e.matmul(pb[:, (h % 4) * 128:(h % 4 + 1) * 128], kbT[:, h, :], khT[:, h, :], start=True, stop=True), reads=[kbT, khT], writes=[pb])
                S.op("dve", lambda e: e.tensor_tensor(tmpf[:], pb[:, :].rearrange("p (j t) -> p j t", j=4), E[:, hg * 4:hg * 4 + 4, 0, :], ALU.mult),
                     reads=[pb, E], writes=[tmpf])
                S.op("dve", lambda e: e.tensor_tensor(P0[:, hg * 4:hg * 4 + 4, :], tmpf[:], RtriT[:, :].unsqueeze(1).to_broadcast([128, 4, 128]), ALU.mult),
                     reads=[tmpf, RtriT], writes=[P0])
                pb = bank()
                for h in hs:
                    S.op("pe", lambda e: e.matmul(pb[:, (h % 4) * 128:(h % 4 + 1) * 128], khT[:, h, :], kbT[:, h, :], start=True, stop=True), reads=[kbT, khT], writes=[pb])
                S.op("dve", lambda e: e.tensor_tensor(tmpf[:], pb[:, :].rearrange("p (j t) -> p j t", j=4), E[:, hg * 4:hg * 4 + 4, 1, :], ALU.mult),
                     reads=[pb, E], writes=[tmpf])
                S.op("dve", lambda e: e.tensor_tensor(PT0[:, hg * 4:hg * 4 + 4, :], tmpf[:], LsT[:, :].unsqueeze(1).to_broadcast([128, 4, 128]), ALU.mult),
                     reads=[tmpf, LsT], writes=[PT0])
                pb = bank()
                for h in hs:
                    S.op("pe", lambda e: e.matmul(pb[:, (h % 4) * 128:(h % 4 + 1) * 128], khT[:, h, :], qsT[:, h, :], start=True, stop=True), reads=[qsT, khT], writes=[pb])
                S.op("dve", lambda e: e.tensor_tensor(tmpf[:], pb[:, :].rearrange("p (j t) -> p j t", j=4), E[:, hg * 4:hg * 4 + 4, 1, :], ALU.mult),
                     reads=[pb, E], writes=[tmpf])
                S.op("dve", lambda e: e.tensor_tensor(atT[:, hg * 4:hg * 4 + 4, :], tmpf[:], LtriT[:, :].unsqueeze(1).to_broadcast([128, 4, 128]), ALU.mult),
                     reads=[tmpf, LtriT], writes=[atT])
                S.op("pool", lambda e: e.tensor_tensor(X0[:, hg * 4:hg * 4 + 4, :], PT0[:, hg * 4:hg * 4 + 4, :],
                                                       c.ident[:, :].unsqueeze(1).to_broadcast([128, 4, 128]), ALU.add),
                     reads=[PT0, c.ident], writes=[X0])
                yield
            cur = 0
            for lev in range(1, 6):
                Pc, PTc, Xc = Pm[cur], PTm[cur], Xm[cur]
                Pn, PTn, Xn = Pm[1 - cur], PTm[1 - cur], Xm[1 - cur]
                for hg in range(2):
                    hs = range(hg * 4, hg * 4 + 4)
                    sl = slice(hg * 4, hg * 4 + 4)
                    pb = bank()
                    for h in hs:
                        S.op("pe", lambda e: e.matmul(pb[:, (h % 4) * 128:(h % 4 + 1) * 128], PTc[:, h, :], Pc[:, h, :], start=True, stop=True), reads=[PTc, Pc], writes=[pb])
                    S.op("act", lambda e: e.copy(Pn[:, sl, :], pb[:, :].rearrange("p (j t) -> p j t", j=4)), reads=[pb], writes=[Pn])
                    if lev < 5:
                        pb = bank()
                        for h in hs:
                            S.op("pe", lambda e: e.matmul(pb[:, (h % 4) * 128:(h % 4 + 1) * 128], Pc[:, h, :], PTc[:, h, :], start=True, stop=True), reads=[PTc, Pc], writes=[pb])
                        S.op("act", lambda e: e.copy(PTn[:, sl, :], pb[:, :].rearrange("p (j t) -> p j t", j=4)), reads=[pb], writes=[PTn])
                    pb = bank()
                    for h in hs:
                        S.op("pe", lambda e: e.matmul(pb[:, (h % 4) * 128:(h % 4 + 1) * 128], Pn[:, h, :], Xc[:, h, :], start=True, stop=True), reads=[Pn, Xc], writes=[pb])
                    S.op("dve", lambda e: e.tensor_tensor(Xn[:, sl, :], pb[:, :].rearrange("p (j t) -> p j t", j=4), Xc[:, sl, :], ALU.add), reads=[pb, Xc], writes=[Xn])
                    yield
                cur = 1 - cur
            X = Xm[cur]
            for hg in range(2):
                sl = slice(hg * 4, hg * 4 + 4)
                pb = bank()
                for h in range(hg * 4, hg * 4 + 4):
                    S.op("pe", lambda e: e.matmul(pb[:, (h % 4) * 128:(h % 4 + 1) * 128], X[:, h, :], vb[:, h, :], start=True, stop=True), reads=[X, vb], writes=[pb])
                S.op("act", lambda e: e.copy(u[:, sl, :], pb[:, :].rearrange("p (j t) -> p j t", j=4)), reads=[pb], writes=[u])
                pb = bank()
                for h in range(hg * 4, hg * 4 + 4):
                    S.op("pe", lambda e: e.matmul(pb[:, (h % 4) * 128:(h % 4 + 1) * 128], kbg[:, h, :], X[:, h, :], start=True, stop=True), reads=[X, kbg], writes=[pb])
                S.op("act", lambda e: e.copy(wT[:, sl, :], pb[:, :].rearrange("p (j t) -> p j t", j=4)), reads=[pb], writes=[wT])
                yield

        def scan_gen(i):
            b = i % 2
            own = (i // 4) in P.own_chunks
            egs = egsL[i % 2]; kdec = kdecL[i % 2]; qdT = qdTL[i % 2]; atT = atTL[i % 2]; u = uL[i % 2]; wT = wTL[i % 2]
            ssq = ssq2; rn = rn2; sq16 = sq8
            for cc in range(2):
                rs = slice(cc * 64, cc * 64 + 64)
                for hg in range(2):
                    sl = slice(hg * 4, hg * 4 + 4)
                    pb = bank()
                    for h in range(hg * 4, hg * 4 + 4):
                        S.op("pe", lambda e: e.matmul(pb[rs, (h % 4) * 128:(h % 4 + 1) * 128], wT[:, h, rs], Sb[:, h, :], start=True, stop=True), reads=[wT, Sb], writes=[pb])
                    S.op("dve", lambda e: e.tensor_tensor(vn[rs, sl, :], u[rs, sl, :], pb[rs, :].rearrange("p (j t) -> p j t", j=4), ALU.subtract), reads=[pb, u], writes=[vn])
                yield
                if own:
                    for hg in range(2):
                        sl = slice(hg * 4, hg * 4 + 4)
                        pb = bank()
                        for h in range(hg * 4, hg * 4 + 4):
                            cs = slice((h % 4) * 128, (h % 4 + 1) * 128)
                            S.op("pe", lambda e: e.matmul(pb[rs, cs], qdT[:, h, rs], Sb[:, h, :], start=True, stop=False), reads=[qdT, Sb], writes=[pb])
                            S.op("pe", lambda e: e.matmul(pb[rs, cs], atT[rs, h, rs], vn[rs, h, :], start=False, stop=True), reads=[atT, vn], writes=[pb])
                        S.op("act", lambda e: e.copy(o[rs, sl, :], pb[rs, :].rearrange("p (j t) -> p j t", j=4)), reads=[pb], writes=[o])
                    yield
                for hg in range(2):
                    sl = slice(hg * 4, hg * 4 + 4)
                    pb = bank()
                    for h in range(hg * 4, hg * 4 + 4):
                        S.op("pe", lambda e: e.matmul(pb[:, (h % 4) * 128:(h % 4 + 1) * 128], kdec[rs, h, :], vn[rs, h, :], start=True, stop=True), reads=[kdec, vn], writes=[pb])
                    S.op("dve", lambda e: e.tensor_tensor(Sf[:, sl, :], Sf[:, sl, :],
                                                          egs[:, 16 + cc * 8 + hg * 4:16 + cc * 8 + hg * 4 + 4].unsqueeze(2).to_broadcast([128, 4, 128]), ALU.mult),
                         reads=[Sf, egs], writes=[Sf])
                    S.op("dve", lambda e: e.tensor_tensor(Sf[:, sl, :], Sf[:, sl, :], pb[:, :].rearrange("p (j t) -> p j t", j=4), ALU.add),
                         reads=[Sf, pb], writes=[Sf])
                    S.op("act", lambda e: e.copy(Sb[:, sl, :], Sf[:, sl, :]), reads=[Sf], writes=[Sb])
                yield
            if own:
                S.dma("sp", zt[:], P.zs.ap[i * 128:(i + 1) * 128, :], zt, P.zs)
                S.op("act", lambda e: e.activation(sq16[:, 0:8, :], o[:], AF.Square), reads=[o], writes=[sq16])
                S.op("dve", lambda e: e.tensor_reduce(ssq[:, 0:8], sq16[:, 0:8, :], AX.X, ALU.add), reads=[sq16], writes=[ssq])
                S.op("act", lambda e: e.activation(rn[:, 0:8], ssq[:, 0:8], AF.Sqrt, bias=c.eps[:, 0:1], scale=1.0 / HD), reads=[ssq, c.eps], writes=[rn])
                S.op("dve", lambda e: e.reciprocal(rn[:, 0:8], rn[:, 0:8]), reads=[rn], writes=[rn])
                S.op("dve", lambda e: e.tensor_tensor(o[:], o[:], rn[:, 0:8].unsqueeze(2).to_broadcast([128, 8, 128]), ALU.mult), reads=[o, rn], writes=[o])
                S.op("pool", lambda e: e.tensor_tensor(o[:], o[:], nw[:, :].unsqueeze(1).to_broadcast([128, 8, 128]), ALU.mult), reads=[o, nw], writes=[o])
                qi = i % 4
                S.op("dve", lambda e: e.tensor_tensor(obc[:, qi, :], o[:, :, :].rearrange("p h d -> p (h d)"), zt[:], ALU.mult), reads=[o, zt], writes=[obc])
                if qi == 3:
                    ch = i // 4
                    S.dma("pool", P.otm.ap[ch * 512:(ch + 1) * 512, 1024:2048].rearrange("(q p) c -> p q c", p=128), obc[:], P.otm, obc)
            yield

        def drain(g):
            for _ in g:
                pass

        drain(prep_gen(0))
        for i in range(NT):
            sg = scan_gen(i)
            pg_ = prep_gen(i + 1) if i + 1 < NT else iter(())
            s_done = p_done = False
            while not (s_done and p_done):
                for _ in range(2):
                    if not p_done:
                        try:
                            next(pg_)
                        except StopIteration:
                            p_done = True
                if not s_done:
                    try:
                        next(sg)
                    except StopIteration:
                        s_done = True


CAP = 128


def own_tiles(P):
    return list(range(P.NO))


def phase_tail1(S, P, c, ps, R):
    tiles = own_tiles(P)
    NO = len(tiles)
    with ExitStack() as st:
        Wo = S.sbuf("t1_wo", [128, KC, D], BF16, st)
        wst = [S.sbuf("t1_wst%d" % i, [128, 2, D], F32, st) for i in range(2)]
        for k0 in range(0, KC, 2):
            w = wst[(k0 // 2) % 2]
            S.dma("sp", w[:], P.w_out.ap[k0 * 128:(k0 + 2) * 128, :].rearrange("(k p) n -> p k n", p=128), w, P.w_out)
            eng = ("dve", "pool")[(k0 // 2) % 2]
            S.op(eng, lambda e: e.tensor_copy(Wo[:, k0:k0 + 2, :], w[:]), reads=[w], writes=[Wo])
        fnw = S.sbuf("t1_fnw", [128, D], F32, st)
        S.dma("sp", fnw[:], P.fnw.ap.partition_broadcast(128), fnw, P.fnw)
        Wr = S.sbuf("t1_wr", [128, KC, 72], F32, st)
        S.dma("sp", Wr[:], P.wr.ap.rearrange("(k p) n -> p k n", p=128), Wr, P.wr)
        brb = S.sbuf("t1_br", [128, 72], F32, st)
        S.dma("sp", brb[:], P.br.ap.partition_broadcast(128), brb, P.br)
        tri = S.sbuf("t1_tri", [128, 128], F32, st)
        onesf = S.sbuf("t1_ones", [128, 128], F32, st)
        S.op("pool", lambda e: e.memset(onesf[:], 1.0), writes=[onesf])
        S.op("pool", lambda e: e.affine_select(tri[:], onesf[:], [[1, 128]], ALU.is_ge, 0.0, base=0, channel_multiplier=-1), reads=[onesf], writes=[tri])
        basei = S.sbuf("t1_basei", [128, 64], I32, st)
        base = S.sbuf("t1_base", [128, 64], F32, st)
        S.op("pool", lambda e: e.iota(basei[:], [[CAP, 64]], base=-1, channel_multiplier=0), writes=[basei])
        S.op("dve", lambda e: e.tensor_copy(base[:], basei[:]), reads=[basei], writes=[base])
        carry = S.sbuf("t1_carry", [128, 64], F32, st)
        S.op("pool", lambda e: e.memset(carry[:], 0.0), writes=[carry])
        otok = [S.sbuf("t1_otok%d" % i, [128, D], BF16, st) for i in range(2)]
        oTt = S.sbuf("t1_oTt", [128, KC, 128], BF16, st)
        idx = S.sbuf("t1_idx", [128, NO], I32, st)
        S.dma("sp", idx[:], P.own_idx.ap, idx, P.own_idx)
        xs = [S.sbuf("t1_x%d" % i, [128, D], F32, st) for i in range(2)]
        h1 = [S.sbuf("t1_h1%d" % i, [128, D], F32, st) for i in range(2)]
        hn = S.sbuf("t1_hn", [128, D], F32, st)
        hnb = [S.sbuf("t1_hnb%d" % i, [128, D], BF16, st) for i in range(2)]
        hT = S.sbuf("t1_hT", [128, KC, 128], F32, st)
        junk = S.sbuf("t1_junk", [128, D], BF16, st)
        ss = S.sbuf("t1_ss", [128, 4], F32, st)
        lgs = [S.sbuf("t1_lg%d" % i, [128, 72], F32, st) for i in range(2)]
        r8 = S.sbuf("t1_r8", [128, 12, 8], F32, st)
        m64 = S.sbuf("t1_m64", [128, 6, 64], F32, st)
        sm = S.sbuf("t1_sm", [128, 16], F32, st)
        loci = [S.sbuf("t1_loci%d_%d" % (i, k), [128, 1], I32, st) for i in range(2) for k in range(2)]

        def front(j):
            b = j % 2
            lg = lgs[b]
            X = xs[b]
            H = h1[b]
            OT = otok[b]
            def gather(jj):
                X_, OT_ = xs[jj % 2], otok[jj % 2]
                S.dma("pool", None, None, X_, P.x, extra_reads=[idx],
                      fn=lambda e: e.indirect_dma_start(X_[:, :], None, P.x.ap[:, :], bass.IndirectOffsetOnAxis(idx[:, jj:jj + 1], 0)))
                S.dma("pool", None, None, OT_, P.otm, extra_reads=[idx],
                      fn=lambda e: e.indirect_dma_start(OT_[:, :], None, P.otm.ap[:, :], bass.IndirectOffsetOnAxis(idx[:, jj:jj + 1], 0)))
            if j == 0:
                gather(0)
            if j + 1 < NO:
                gather(j + 1)
            for k0 in range(0, KC, 8):
                pb = ps[6 + (k0 // 8) % 2]
                pv = pb.ap.bitcast(BF16)
                for k in range(k0, k0 + 8):
                    S.op("pe", lambda e: e.transpose(pv[:, (k - k0) * 128:(k - k0 + 1) * 128], OT[:, k * 128:(k + 1) * 128], c.ident[:]), reads=[OT, c.ident], writes=[pb])
                S.op("act", lambda e: e.copy(oTt[:, k0:k0 + 8, :], pv[:, 0:1024].rearrange("p (k t) -> p k t", k=8)), reads=[pb], writes=[oTt])
            for nc4 in range(4):
                pb = ps[nc4 % 2]
                for k in range(KC):
                    S.op("pe", lambda e: e.matmul(pb[:, :], oTt[:, k, :], Wo[:, k, nc4 * 512:(nc4 + 1) * 512],
                                                  start=(k == 0), stop=(k == KC - 1)), reads=[oTt, Wo], writes=[pb])
                S.op("dve", lambda e: e.tensor_tensor(H[:, nc4 * 512:(nc4 + 1) * 512], pb[:, :], X[:, nc4 * 512:(nc4 + 1) * 512], ALU.add),
                     reads=[pb, X], writes=[H])
            S.dma("pool", P.h1.ap[j * 128:(j + 1) * 128, :], H[:], P.h1, H)
            yield
            S.op("act", lambda e: e.activation(junk[:], H[:], AF.Square, accum_out=ss[:, 0:1]), reads=[H], writes=[junk, ss])
            S.op("act", lambda e: e.activation(ss[:, 1:2], ss[:, 0:1], AF.Sqrt, bias=c.eps[:, 0:1], scale=1.0 / D), reads=[ss, c.eps], writes=[ss])
            S.op("dve", lambda e: e.reciprocal(ss[:, 1:2], ss[:, 1:2]), reads=[ss], writes=[ss])
            S.op("dve", lambda e: e.scalar_tensor_tensor(hn[:], H[:], ss[:, 1:2], fnw[:], ALU.mult, ALU.mult), reads=[H, ss, fnw], writes=[hn])
            HB = hnb[b]
            S.op("act", lambda e: e.copy(HB[:], hn[:]), reads=[hn], writes=[HB])
            yield
            for k0 in range(0, KC, 4):
                pb = ps[2 + (k0 // 4) % 2]
                for k in range(k0, k0 + 4):
                    S.op("pe", lambda e: e.transpose(pb[:, (k - k0) * 128:(k - k0 + 1) * 128], hn[:, k * 128:(k + 1) * 128], c.identf[:]),
                         reads=[hn, c.identf], writes=[pb])
                eng = ("act", "dve")[(k0 // 4) % 2]
                if eng == "act":
                    S.op("act", lambda e: e.copy(hT[:, k0:k0 + 4, :], pb[:, :].rearrange("p (k t) -> p k t", k=4)), reads=[pb], writes=[hT])
                else:
                    S.op("dve", lambda e: e.tensor_copy(hT[:, k0:k0 + 4, :], pb[:, :].rearrange("p (k t) -> p k t", k=4)), reads=[pb], writes=[hT])
            pl = ps[4]
            for k in range(KC):
                S.op("pe", lambda e: e.matmul(pl[:, 0:72], hT[:, k, :], Wr[:, k, :], start=(k == 0), stop=(k == KC - 1)), reads=[hT, Wr], writes=[pl])
            S.op("dve", lambda e: e.tensor_tensor(lg[:], pl[:, 0:72], brb[:], ALU.add), reads=[pl, brb], writes=[lg])
            yield

        def back(j):
            b = j % 2
            lg = lgs[b]
            HB = hnb[b]
            LG = lg[:, 0:8]
            LE = lg[:, 8:72]
            gmax, ngmax, se, pg, v12, w1, w2 = (sm[:, i:i + 1] for i in range(7))
            ohg, ex, ing, v8, sel1, sel2 = (r8[:, i, :] for i in range(6))
            S.op("dve", lambda e: e.reduce_max(gmax, LG, AX.X), reads=[lg], writes=[sm])
            S.op("dve", lambda e: e.tensor_scalar(ohg, LG, gmax, None, ALU.is_equal), reads=[lg, sm], writes=[r8])
            S.op("dve", lambda e: e.tensor_scalar(ngmax, gmax, -1.0, None, ALU.mult), reads=[sm], writes=[sm])
            S.op("act", lambda e: e.activation(ex, LG, AF.Exp, bias=ngmax, accum_out=se), reads=[lg, sm], writes=[r8, sm])
            S.op("dve", lambda e: e.reciprocal(pg, se), reads=[sm], writes=[sm])
            t64 = m64[:, 0, :]
            S.op("dve", lambda e: e.tensor_tensor(t64.rearrange("p (g j) -> p g j", g=8), LE.rearrange("p (g j) -> p g j", g=8),
                                                  ohg.unsqueeze(2).to_broadcast([128, 8, 8]), ALU.mult), reads=[lg, r8], writes=[m64])
            S.op("dve", lambda e: e.tensor_reduce(ing, t64.rearrange("p (g j) -> p j g", g=8), AX.X, ALU.add), reads=[m64], writes=[r8])
            S.op("dve", lambda e: e.max(v8, ing), reads=[r8], writes=[r8])
            S.op("dve", lambda e: e.tensor_scalar(sel1, ing, r8[:, 3, 0:1], None, ALU.is_equal), reads=[r8], writes=[r8])
            S.op("dve", lambda e: e.tensor_scalar(sel2, ing, r8[:, 3, 1:2], None, ALU.is_equal), reads=[r8], writes=[r8])
            S.op("dve", lambda e: e.tensor_tensor(v12, r8[:, 3, 0:1], r8[:, 3, 1:2], ALU.subtract), reads=[r8], writes=[sm])
            S.op("act", lambda e: e.activation(v12, v12, AF.Sigmoid), reads=[sm], writes=[sm])
            S.op("dve", lambda e: e.tensor_tensor(w1, v12, pg, ALU.mult), reads=[sm], writes=[sm])
            S.op("dve", lambda e: e.tensor_tensor(w2, pg, w1, ALU.subtract), reads=[sm], writes=[sm])
            S.op("dve", lambda e: e.tensor_copy(R.w[:, j, 0:1], w1), reads=[sm], writes=[R.w])
            S.op("dve", lambda e: e.tensor_copy(R.w[:, j, 1:2], w2), reads=[sm], writes=[R.w])
            yield
            m1, m2, mk_, val, tmp_ = (m64[:, i, :] for i in range(1, 6))
            for mm, sel in ((m1, sel1), (m2, sel2)):
                S.op("dve", lambda e: e.tensor_tensor(mm.rearrange("p (g j) -> p g j", g=8), ohg.unsqueeze(2).to_broadcast([128, 8, 8]),
                                                      sel.unsqueeze(1).to_broadcast([128, 8, 8]), ALU.mult), reads=[r8], writes=[m64])
            S.op("dve", lambda e: e.tensor_tensor(mk_, m1, m2, ALU.add), reads=[m64], writes=[m64])
            pc = ps[5]
            S.op("pe", lambda e: e.matmul(pc[:, 0:64], tri[:], mk_, start=True, stop=True), reads=[tri, m64], writes=[pc])
            S.op("pe", lambda e: e.matmul(pc[:, 64:128], onesf[:], mk_, start=True, stop=True), reads=[onesf, m64], writes=[pc])
            S.op("dve", lambda e: e.tensor_tensor(val, pc[:, 0:64], carry[:], ALU.add), reads=[pc, carry], writes=[m64])
            S.op("dve", lambda e: e.tensor_scalar(val, val, float(CAP), None, ALU.min), reads=[m64], writes=[m64])
            S.op("dve", lambda e: e.tensor_tensor(val, val, base[:], ALU.add), reads=[m64, base], writes=[m64])
            S.op("dve", lambda e: e.tensor_tensor(carry[:], carry[:], pc[:, 64:128], ALU.add), reads=[pc, carry], writes=[carry])
            yield
            for kk, mm in enumerate((m1, m2)):
                S.op("dve", lambda e: e.tensor_tensor(tmp_, mm, val, ALU.mult), reads=[m64], writes=[m64])
                S.op("dve", lambda e: e.reduce_sum(sm[:, 8 + kk:9 + kk], tmp_, AX.X), reads=[m64], writes=[sm])
                li = loci[b * 2 + kk]
                S.op("dve", lambda e: e.tensor_copy(li[:], sm[:, 8 + kk:9 + kk]), reads=[sm], writes=[li])
                S.op("dve", lambda e: e.tensor_copy(R.loc[:, j, kk:kk + 1], li[:]), reads=[li], writes=[R.loc])
                S.dma("pool", None, None, P.xdisp, HB, extra_reads=[li],
                      fn=lambda e: e.indirect_dma_start(P.xdisp.ap[:, :], bass.IndirectOffsetOnAxis(li[:, 0:1], 0), HB[:, :], None))
            yield

        def drain(g):
            for _ in g:
                pass

        drain(front(0))
        for j in range(NO):
            bg = back(j)
            fg = front(j + 1) if j + 1 < NO else iter(())
            b_done = f_done = False
            while not (b_done and f_done):
                if not f_done:
                    try:
                        next(fg)
                    except StopIteration:
                        f_done = True
                if not b_done:
                    try:
                        next(bg)
                    except StopIteration:
                        b_done = True


def phase_tail2(S, P, c, ps):
    with ExitStack() as st:
        wst = [S.sbuf("t2_wst%d" % i, [128, 8 * 512], F32, st) for i in range(5)]
        wbf = [S.sbuf("t2_wbf%d" % i, [128, 8 * 512], BF16, st) for i in range(5)]
        Xe = [S.sbuf("t2_xe%d" % i, [128, D], BF16, st) for i in range(2)]
        XT = S.sbuf("t2_xT", [128, KC, 128], BF16, st)
        hs = S.sbuf("t2_hs", [128, 512], F32, st)
        hb = S.sbuf("t2_hb", [128, 512], BF16, st)
        HT = S.sbuf("t2_hT", [128, 4, 128], BF16, st)
        ys = [S.sbuf("t2_y%d" % i, [128, D], F32, st) for i in range(2)]
        pcs = [0]

        def piece(src_ap):
            i = pcs[0] % 5
            eng = ("dve", "pool", "act")[pcs[0] % 3]
            pcs[0] += 1
            S.dma("sp", wst[i][:], src_ap, wst[i], P.moe_w)
            if eng == "act":
                S.op("act", lambda e: e.copy(wbf[i][:], wst[i][:]), reads=[wst[i]], writes=[wbf[i]])
            else:
                S.op(eng, lambda e: e.tensor_copy(wbf[i][:], wst[i][:]), reads=[wst[i]], writes=[wbf[i]])
            return wbf[i]

        for ex in range(64):
            X = Xe[ex % 2]
            S.dma("pool", X[:], P.xdisp.ap[ex * CAP:(ex + 1) * CAP, :], X, P.xdisp)
            for k0 in range(0, KC, 8):
                pb = ps[6 + (k0 // 8) % 2]
                pv = pb.ap.bitcast(BF16)
                for k in range(k0, k0 + 8):
                    S.op("pe", lambda e: e.transpose(pv[:, (k - k0) * 128:(k - k0 + 1) * 128], X[:, k * 128:(k + 1) * 128], c.ident[:]), reads=[X, c.ident], writes=[pb])
                S.op("act", lambda e: e.copy(XT[:, k0:k0 + 8, :], pv[:, 0:1024].rearrange("p (k t) -> p k t", k=8)), reads=[pb], writes=[XT])
            for wi, (W_, pb) in enumerate(((P.wg, ps[0]), (P.wu, ps[1]))):
                for half in range(2):
                    wb = piece(W_.ap[ex][half * 1024:(half + 1) * 1024, :].rearrange("(k p) n -> p k n", p=128))
                    for k in range(8):
                        kk = half * 8 + k
                        S.op("pe", lambda e: e.matmul(pb[:, :], XT[:, kk, :], wb[:, k * 512:(k + 1) * 512], start=(kk == 0), stop=(kk == KC - 1)),
                             reads=[XT, wb], writes=[pb])
            S.op("act", lambda e: e.activation(hs[:], ps[0][:, :], AF.Silu), reads=[ps[0]], writes=[hs])
            S.op("dve", lambda e: e.tensor_tensor(hb[:], hs[:], ps[1][:, :], ALU.mult), reads=[hs, ps[1]], writes=[hb])
            pb = ps[6]
            pv = pb.ap.bitcast(BF16)
            for k in range(4):
                S.op("pe", lambda e: e.transpose(pv[:, k * 128:(k + 1) * 128], hb[:, k * 128:(k + 1) * 128], c.ident[:]), reads=[hb, c.ident], writes=[pb])
            S.op("act", lambda e: e.copy(HT[:, :, :], pv[:, 0:512].rearrange("p (k t) -> p k t", k=4)), reads=[pb], writes=[HT])
            Y = ys[ex % 2]
            for half in range(2):
                wb = piece(P.wd.ap[ex][half * 256:(half + 1) * 256, :].rearrange("(k p) n -> p k n", p=128))
                for k in range(2):
                    kk = half * 2 + k
                    for n4 in range(4):
                        pby = ps[2 + n4]
                        S.op("pe", lambda e: e.matmul(pby[:, :], HT[:, kk, :], wb[:, k * 2048 + n4 * 512:k * 2048 + (n4 + 1) * 512], start=(kk == 0), stop=(kk == 3)),
                             reads=[HT, wb], writes=[pby])
            for n4 in range(4):
                if n4 % 2 == 0:
                    S.op("act", lambda e: e.copy(Y[:, n4 * 512:(n4 + 1) * 512], ps[2 + n4][:, :]), reads=[ps[2 + n4]], writes=[Y])
                else:
                    S.op("dve", lambda e: e.tensor_copy(Y[:, n4 * 512:(n4 + 1) * 512], ps[2 + n4][:, :]), reads=[ps[2 + n4]], writes=[Y])
            S.dma("pool", P.yall.ap[ex * CAP:(ex + 1) * CAP, :], Y[:], P.yall, Y)


def phase_tail3(S, P, c, ps, R):
    tiles = own_tiles(P)
    with ExitStack() as st:
        fw = S.sbuf("t3_fw", [128, D], F32, st)
        S.dma("sp", fw[:], P.finw.ap.partition_broadcast(128), fw, P.finw)
        h1 = [S.sbuf("t3_h%d" % i, [128, D], F32, st) for i in range(2)]
        y1 = [S.sbuf("t3_y1%d" % i, [128, D], F32, st) for i in range(2)]
        y2 = [S.sbuf("t3_y2%d" % i, [128, D], F32, st) for i in range(2)]
        ot = [S.sbuf("t3_o%d" % i, [128, D], F32, st) for i in range(2)]
        junk = S.sbuf("t3_junk", [128, D], BF16, st)
        ss = S.sbuf("t3_ss", [128, 2], F32, st)
        for j in range(len(tiles)):
            b = j % 2
            H, Y1, Y2, O = h1[b], y1[b], y2[b], ot[b]
            S.dma("sp", H[:], P.h1.ap[j * 128:(j + 1) * 128, :], H, P.h1)
            for kk, Y in enumerate((Y1, Y2)):
                S.dma("pool", None, None, Y, P.yall, extra_reads=[R.loc],
                      fn=lambda e: e.indirect_dma_start(Y[:, :], None, P.yall.ap[:, :], bass.IndirectOffsetOnAxis(R.loc[:, j, kk:kk + 1], 0)))
            S.op("dve", lambda e: e.scalar_tensor_tensor(H[:], Y1[:], R.w[:, j, 0:1], H[:], ALU.mult, ALU.add), reads=[Y1, R.w, H], writes=[H])
            S.op("dve", lambda e: e.scalar_tensor_tensor(H[:], Y2[:], R.w[:, j, 1:2], H[:], ALU.mult, ALU.add), reads=[Y2, R.w, H], writes=[H])
            S.op("act", lambda e: e.activation(junk[:], H[:], AF.Square, accum_out=ss[:, 0:1]), reads=[H], writes=[junk, ss])
            S.op("act", lambda e: e.activation(ss[:, 1:2], ss[:, 0:1], AF.Sqrt, bias=c.eps[:, 0:1], scale=1.0 / D), reads=[ss, c.eps], writes=[ss])
            S.op("dve", lambda e: e.reciprocal(ss[:, 1:2], ss[:, 1:2]), reads=[ss], writes=[ss])
            S.op("dve", lambda e: e.scalar_tensor_tensor(O[:], H[:], ss[:, 1:2], fw[:], ALU.mult, ALU.mult), reads=[H, ss, fw], writes=[O])
            S.dma("sp", P.out.ap[j * 128:(j + 1) * 128, :], O[:], P.out, O)


def make_in_maps(inputs, T, n_batch, own_div=2):
    f = lambda a: np.ascontiguousarray(np.asarray(a))
    l = 0
    shared = {
        "anw": f(np.asarray(inputs["attn_norm_w"])[l].reshape(KC, 128).T),
        "w_in": f(np.asarray(inputs["w_in"])[l]),
        "convw": f(np.asarray(inputs["gdn_conv_w"])[l].reshape(4, 24, 128).transpose(2, 1, 0)),
        "cmp_wk": f(np.asarray(inputs["cmp_wk"])[l]),
        "cmp_wv": f(np.asarray(inputs["cmp_wv"])[l]),
        "cmp_pe": f(np.stack([np.asarray(inputs["cmp_pek"])[l].T, np.asarray(inputs["cmp_pev"])[l].T], axis=1)),
        "gpar": f(np.concatenate([np.asarray(inputs["gdn_a_log"])[l], np.asarray(inputs["gdn_dt_bias"])[l]])),
        "gnw": f(np.asarray(inputs["gdn_norm_w"])[l]),
        "w_out": f(np.asarray(inputs["w_out"])[l]),
        "fnw": f(np.asarray(inputs["ffn_norm_w"])[l]),
        "wr": f(np.concatenate([np.asarray(inputs["router_group_w"])[l], np.asarray(inputs["router_expert_w"])[l]], axis=1)),
        "br": f(np.concatenate([np.asarray(inputs["router_group_b"])[l], np.asarray(inputs["router_expert_b"])[l]])),
        "wg": f(np.asarray(inputs["moe_w_gate"])[l]),
        "wu": f(np.asarray(inputs["moe_w_up"])[l]),
        "wd": f(np.asarray(inputs["moe_w_down"])[l]),
        "finw": f(np.asarray(inputs["final_norm_w"])),
    }
    maps, owns = [], []
    nch = T // 512
    for b in range(n_batch):
        for g in range(own_div):
            chunks = [ch for ch in range(nch) if ch % own_div == g]
            idx = np.concatenate([np.arange(ch * 512, (ch + 1) * 512) for ch in chunks]).astype(np.int32)
            m = dict(shared)
            m["x"] = f(np.asarray(inputs["x"])[b, :T])
            m["pos"] = f(np.asarray(inputs["positions"])[b, :T].astype(np.int32))
            m["own_idx"] = f(idx.reshape(-1, 128).T)
            maps.append(m)
            owns.append((b, idx))
    return maps, owns


_NC_CACHE = {}


def kernel(**inputs):
    T = 4096
    B = 4
    if "nc" not in _NC_CACHE:
        _NC_CACHE["nc"] = build(T)
    nc = _NC_CACHE["nc"]
    maps, owns = make_in_maps(inputs, T, B)
    res = run_bass_kernel_spmd(nc, maps, core_ids=list(range(8)))
    out = np.zeros((B, T, D), np.float32)
    for (b, idx), r in zip(owns, res.results):
        out[b, idx] = r["out"]
    return out
```

```python
from contextlib import ExitStack
import numpy as np
import concourse.bass as bass
import concourse.mybir as mybir
from concourse.bass_utils import run_bass_kernel_spmd

F32 = mybir.dt.float32
BF16 = mybir.dt.bfloat16
I32 = mybir.dt.int32
U32 = mybir.dt.uint32
ALU = mybir.AluOpType
AF = mybir.ActivationFunctionType
AX = mybir.AxisListType

D = 2048
KC = D // 128
HD = 128
NEG = -30000.0
EPS = 1e-6


class Buf:
    __slots__ = ("name", "w", "r", "dsem", "dcnt", "ap", "is_dram")

    def __init__(self, name, ap=None, is_dram=False):
        self.name = name
        self.w = None
        self.r = {}
        self.dsem = None
        self.dcnt = 0
        self.ap = ap
        self.is_dram = is_dram

    def __getitem__(self, k):
        return self.ap[k]


class Sched:
    def __init__(self, nc, stack):
        self.nc = nc
        self.stack = stack
        self.eng = {"pe": nc.tensor, "act": nc.scalar, "dve": nc.vector,
                    "pool": nc.gpsimd, "sp": nc.sync}
        self.sem = {}
        self.cnt = {}
        for e in ("pe", "act", "dve", "pool"):
            self.sem[e] = stack.enter_context(nc.semaphore("s_" + e))
            self.cnt[e] = 0
        self.waited = {}
        self.n_ins = 0
        self.n_dsem = 0
        self.dbufs = []

    def sbuf(self, name, shape, dtype, stack=None):
        st = stack if stack is not None else self.stack
        t = st.enter_context(self.nc.sbuf_tensor(name, list(shape), dtype))
        return Buf(name, t.ap())

    def psum(self, name, shape, dtype, stack=None):
        st = stack if stack is not None else self.stack
        t = st.enter_context(self.nc.psum_tensor(name, list(shape), dtype))
        return Buf(name, t.ap())

    def dram(self, name, shape, dtype, kind="Internal"):
        t = self.nc.dram_tensor(name, list(shape), dtype, kind=kind)
        return Buf(name, t.ap(), is_dram=True)

    def _wait(self, e, ev):
        if ev[0] == "eng":
            sem, val = self.sem[ev[1]], ev[2]
        else:
            sem, val = ev[1].dsem, ev[1].dcnt
        key = (e, id(sem))
        if self.waited.get(key, 0) >= val:
            return
        self.waited[key] = val
        self.eng[e].wait_ge(sem, val)

    def _deps(self, e, reads, writes, dma=False):
        evs = []
        for b in reads:
            if b.w is not None:
                evs.append(b.w)
        for b in writes:
            if b.w is not None:
                evs.append(b.w)
            for ev in b.r.values():
                evs.append(ev)
        for ev in evs:
            if (not dma) and ev[0] == "eng" and ev[1] == e and e == "pe":
                continue
            self._wait(e, ev)

    def op(self, e, fn, reads=(), writes=()):
        self._deps(e, reads, writes)
        ins = fn(self.eng[e])
        self.cnt[e] += 1
        ins.then_inc(self.sem[e], 1)
        ev = ("eng", e, self.cnt[e])
        for b in writes:
            b.w = ev
            b.r = {}
        for b in reads:
            if b not in writes:
                b.r[e] = ev
        self.n_ins += 1
        return ins

    def dma(self, q, out_ap, in_ap, out_buf, in_buf, extra_reads=(), fn=None):
        sb = out_buf if not out_buf.is_dram else in_buf
        assert not sb.is_dram
        if sb.dsem is None:
            sb.dsem = self.stack.enter_context(self.nc.semaphore("d%d" % self.n_dsem))
            self.n_dsem += 1
            self.dbufs.append(sb)
        self._deps(q, [in_buf] + list(extra_reads), [out_buf], dma=True)
        if fn is None:
            ins = self.eng[q].dma_start(out=out_ap, in_=in_ap)
        else:
            ins = fn(self.eng[q])
        ins.then_inc(sb.dsem, 16)
        sb.dcnt += 16
        ev = ("dma", sb)
        out_buf.w = ev
        out_buf.r = {}
        in_buf.r["dma%d" % id(sb)] = ev
        for b in extra_reads:
            b.r["dma%d" % id(sb)] = ev
        self.n_ins += 1
        return ins

    def barrier(self):
        for e in ("sp", "pool", "act", "dve", "pe"):
            for e2 in ("pe", "act", "dve", "pool"):
                if e2 != e and self.cnt[e2] > 0:
                    self._wait(e, ("eng", e2, self.cnt[e2]))
            for b in self.dbufs:
                self._wait(e, ("dma", b))

    def wait_all(self, e, bufs):
        for b in bufs:
            if b.w is not None:
                self._wait(e, b.w)
            for ev in b.r.values():
                self._wait(e, ev)


class Prog:
    pass


def build_consts(S, P, st):
    c = Prog()
    c.identf = S.sbuf("identf", [128, 128], F32, st)
    c.ident = S.sbuf("ident", [128, 128], BF16, st)
    S.op("pool", lambda e: e.memset(c.identf[:], 0.0), writes=[c.identf])
    S.op("pool", lambda e: e.affine_select(c.identf[:], c.identf[:], [[-1, 128]], ALU.not_equal, 1.0,
                                           base=0, channel_multiplier=1),
         reads=[c.identf], writes=[c.identf])
    S.op("dve", lambda e: e.tensor_copy(c.ident[:], c.identf[:]), reads=[c.identf], writes=[c.ident])
    return c


def phase_a0(S, P, c, ps):
    T, NT = P.T, P.NT
    with ExitStack() as st:
        xs = [S.sbuf("a0_x%d" % i, [128, D], F32, st) for i in range(2)]
        junk = S.sbuf("a0_junk", [128, D], BF16, st)
        xh = [S.sbuf("a0_xh%d" % i, [128, D], BF16, st) for i in range(2)]
        xt = [S.sbuf("a0_xt%d" % i, [128, KC, 128], BF16, st) for i in range(2)]
        ss = S.sbuf("a0_ss", [128, 2], F32, st)
        S.dma("sp", xs[0][:], P.x[0:128, :], xs[0], P.x)
        for i in range(NT):
            b = i % 2
            if i + 1 < NT:
                S.dma("sp", xs[1 - b][:], P.x[(i + 1) * 128:(i + 2) * 128, :], xs[1 - b], P.x)
            X = xs[b]
            S.op("act", lambda e: e.activation(junk[:], X[:], AF.Square, accum_out=ss[:, 0:1]),
                 reads=[X], writes=[junk, ss])
            S.op("act", lambda e: e.activation(ss[:, 1:2], ss[:, 0:1], AF.Sqrt, bias=c.eps[:, 0:1], scale=1.0 / D),
                 reads=[ss, c.eps], writes=[ss])
            S.op("dve", lambda e: e.reciprocal(ss[:, 1:2], ss[:, 1:2]), reads=[ss], writes=[ss])
            S.op("dve", lambda e: e.tensor_scalar(xh[b][:], X[:], ss[:, 1:2], None, ALU.mult),
                 reads=[X, ss], writes=[xh[b]])
            for half in range(2):
                pb = ps[half]
                pv = pb.ap.bitcast(BF16)
                for j in range(8):
                    k = half * 8 + j
                    S.op("pe", lambda e: e.transpose(pv[:, j * 128:(j + 1) * 128], xh[b][:, k * 128:(k + 1) * 128], c.ident[:]),
                         reads=[xh[b], c.ident], writes=[pb])
                eng = "act" if half == 0 else "dve"
                if eng == "act":
                    S.op("act", lambda e: e.copy(xt[b][:, half * 8:(half + 1) * 8, :], pv[:, 0:1024].rearrange("p (k t) -> p k t", k=8)),
                         reads=[pb], writes=[xt[b]])
                else:
                    S.op("dve", lambda e: e.tensor_copy(xt[b][:, half * 8:(half + 1) * 8, :], pv[:, 0:1024].rearrange("p (k t) -> p k t", k=8)),
                         reads=[pb], writes=[xt[b]])
            S.dma("pool", P.xT[:, :, i * 128:(i + 1) * 128], xt[b][:], P.xT, xt[b])


def rope_tables(S, P, c, st):
    T = P.T
    cosT = S.sbuf("rp_cos", [32, T], F32, st)
    sinT = S.sbuf("rp_sin", [32, T], F32, st)
    st2 = ExitStack()
    posi = S.sbuf("rp_posi", [32, T], I32, st2)
    ang = S.sbuf("rp_ang", [32, T], F32, st2)
    tmp = S.sbuf("rp_tmp", [32, T], F32, st2)
    ki = S.sbuf("rp_ki", [32, T], I32, st2)
    S.dma("sp", posi[:], P.pos.ap.partition_broadcast(32), posi, P.pos)
    S.op("dve", lambda e: e.tensor_copy(ang[:], posi[:]), reads=[posi], writes=[ang])
    S.op("dve", lambda e: e.tensor_scalar(ang[:], ang[:], c.invf[0:32, 0:1], None, ALU.mult), reads=[ang, c.invf], writes=[ang])
    TWO_PI = 2.0 * np.pi

    def reduced_sin(out, shift, sign_ap):
        S.op("dve", lambda e: e.tensor_scalar(tmp[:], ang[:], 1.0 / TWO_PI, (shift / TWO_PI) + 0.5, ALU.mult, ALU.add), reads=[ang], writes=[tmp])
        S.op("dve", lambda e: e.tensor_copy(ki[:], tmp[:]), reads=[tmp], writes=[ki])
        S.op("dve", lambda e: e.tensor_copy(tmp[:], ki[:]), reads=[ki], writes=[tmp])
        S.op("dve", lambda e: e.scalar_tensor_tensor(tmp[:], tmp[:], -TWO_PI, ang[:], ALU.mult, ALU.add), reads=[tmp, ang], writes=[tmp])
        S.op("dve", lambda e: e.tensor_scalar(tmp[:], tmp[:], float(shift), None, ALU.add), reads=[tmp], writes=[tmp])
        S.op("dve", lambda e: e.tensor_scalar(out[:], tmp[:], float(np.pi), -TWO_PI, ALU.is_gt, ALU.mult), reads=[tmp], writes=[out])
        S.op("dve", lambda e: e.tensor_tensor(tmp[:], tmp[:], out[:], ALU.add), reads=[tmp, out], writes=[tmp])
        S.op("dve", lambda e: e.tensor_scalar(out[:], tmp[:], float(-np.pi), TWO_PI, ALU.is_lt, ALU.mult), reads=[tmp], writes=[out])
        S.op("dve", lambda e: e.tensor_tensor(tmp[:], tmp[:], out[:], ALU.add), reads=[tmp, out], writes=[tmp])
        S.op("dve", lambda e: e.tensor_scalar(tmp[:], tmp[:], float(np.pi), float(-np.pi), ALU.min, ALU.max), reads=[tmp], writes=[tmp])
        S.op("act", lambda e: e.activation(out[:], tmp[:], AF.Sin), reads=[tmp], writes=[out])
        if sign_ap is not None:
            S.op("dve", lambda e: e.tensor_scalar(out[:], out[:], sign_ap, None, ALU.mult), reads=[out, c.sgn], writes=[out])

    reduced_sin(sinT, 0.0, c.sgn[0:32, 0:1])
    reduced_sin(cosT, np.pi / 2.0, None)
    S.barrier()
    st2.close()
    return cosT, sinT


def phase_a1(S, P, c, ps):
    T, NT = P.T, P.NT
    NCH = T // 512
    with ExitStack() as st:
        cosT, sinT = rope_tables(S, P, c, st)
        WbL = [S.sbuf("a1_wb%d" % i, [128, KC, 1536], BF16, st) for i in range(2)]
        gstate = {"gi": 0}
        Wf = [S.sbuf("a1_wf%d" % i, [128, KC, 128], F32, st) for i in range(2)]
        xtc = [S.sbuf("a1_xt%d" % i, [128, KC, 512], BF16, st) for i in range(2)]
        qf = [S.sbuf("a1_qf%d" % i, [128, 512], BF16, st) for i in range(2)]
        t1 = S.sbuf("a1_t1", [32, 512], F32, st)
        t2 = S.sbuf("a1_t2", [32, 512], F32, st)
        xc = [S.sbuf("a1_xc%d" % i, [128, 515], F32, st) for i in range(2)]
        yc = [S.sbuf("a1_yc%d" % i, [128, 512], F32, st) for i in range(2)]
        carry = S.sbuf("a1_carry", [128, 24, 3], F32, st)
        tmo = [S.sbuf("a1_tmo%d" % i, [128, 512], F32, st) for i in range(2)]
        tmb = [S.sbuf("a1_tmb%d" % i, [128, 512], BF16, st) for i in range(2)]
        S.op("pool", lambda e: e.memset(carry[:], 0.0), writes=[carry])
        wcnt = [0]
        rr = [0]

        def w_pieces(col0, ncols):
            return [(col0, j0, min(128, ncols - j0)) for j0 in range(0, ncols, 128)]

        def load_piece(Wb, col0, j0, n):
            wf = Wf[wcnt[0] % 2]
            wcnt[0] += 1
            S.dma("sp", wf[:, :, 0:n], P.w_in.ap[:, col0 + j0:col0 + j0 + n].rearrange("(k p) n -> p k n", p=128), wf, P.w_in)
            S.op("pool", lambda e: e.tensor_tensor(Wb[:, :, j0:j0 + n], wf[:, :, 0:n],
                                                   c.anw[:, :].unsqueeze(2).to_broadcast([128, KC, n]), ALU.mult),
                 reads=[wf, c.anw], writes=[Wb])

        def load_x(ch, b):
            S.dma("sp", xtc[b][:], P.xT[:, :, ch * 512:(ch + 1) * 512], xtc[b], P.xT)

        def fm_mm(pb, j0, xb):
            Wb = gstate["Wb"]
            for k in range(KC):
                S.op("pe", lambda e: e.matmul(pb[:, :], Wb[:, k, j0:j0 + 128], xb[:, k, :], start=(k == 0), stop=(k == KC - 1)),
                     reads=[Wb, xb], writes=[pb])

        def group(col0, ncols, blocks, nxt=None):
            gi = gstate["gi"]
            gstate["gi"] += 1
            Wb = WbL[gi % 2]
            gstate["Wb"] = Wb
            if gi == 0:
                for pc_ in w_pieces(col0, ncols):
                    load_piece(Wb, *pc_)
            pend = w_pieces(*nxt) if nxt is not None else []
            per = (len(pend) + NCH - 1) // NCH if pend else 0
            load_x(0, 0)
            for ch in range(NCH):
                b = ch % 2
                if ch + 1 < NCH:
                    load_x(ch + 1, 1 - b)
                for _ in range(per):
                    if pend:
                        load_piece(WbL[(gi + 1) % 2], *pend.pop(0))
                xb = xtc[b]
                tsl = slice(ch * 512, (ch + 1) * 512)
                for blk in blocks:
                    kind = blk[0]
                    r = rr[0] % 2
                    rr[0] += 1
                    pb = ps[2 + r]
                    if kind == "rope":
                        _, j0, dst = blk
                        fm_mm(pb, j0, xb)
                        q = qf[r]
                        S.op("act", lambda e: e.copy(q[:], pb[:, :]), reads=[pb], writes=[q])
                        pr = ps[4 + r]
                        S.op("pe", lambda e: e.matmul(pr[0:32, :], c.pswap[:, :], q[:], start=True, stop=True),
                             reads=[c.pswap, q], writes=[pr])
                        S.op("dve", lambda e: e.tensor_tensor(t1[:], pr[0:32, :], sinT[:, tsl], ALU.mult), reads=[pr, sinT], writes=[t1])
                        S.op("dve", lambda e: e.tensor_tensor(t2[:], pb[0:32, :], cosT[:, tsl], ALU.mult), reads=[pb, cosT], writes=[t2])
                        S.op("dve", lambda e: e.tensor_tensor(q[0:32, :], t1[:], t2[:], ALU.add), reads=[t1, t2], writes=[q])
                        S.dma("pool", dst[:, tsl], q[:], dst.buf, q)
                    elif kind == "fm":
                        _, j0, dst = blk
                        fm_mm(pb, j0, xb)
                        q = qf[r]
                        S.op("act", lambda e: e.copy(q[:], pb[:, :]), reads=[pb], writes=[q])
                        S.dma("pool", dst[:, tsl], q[:], dst.buf, q)
                    elif kind == "gdn":
                        _, j0, gb, dst = blk
                        fm_mm(pb, j0, xb)
                        X = xc[r]
                        Y = yc[r]
                        S.op("act", lambda e: e.copy(X[:, 0:3], carry[:, gb, :]), reads=[carry], writes=[X])
                        S.op("act", lambda e: e.copy(X[:, 3:515], pb[:, :]), reads=[pb], writes=[X])
                        S.op("act", lambda e: e.copy(carry[:, gb, :], X[:, 512:515]), reads=[X], writes=[carry])
                        S.op("dve", lambda e: e.tensor_scalar(Y[:], X[:, 0:512], c.convw[:, gb, 0:1], None, ALU.mult), reads=[X, c.convw], writes=[Y])
                        for j in range(1, 4):
                            S.op("dve", lambda e: e.scalar_tensor_tensor(Y[:], X[:, j:j + 512], c.convw[:, gb, j:j + 1], Y[:], ALU.mult, ALU.add),
                                 reads=[X, c.convw, Y], writes=[Y])
                        q = qf[r]
                        S.op("act", lambda e: e.activation(q[:], Y[:], AF.Silu), reads=[Y], writes=[q])
                        S.dma("pool", dst[:, tsl], q[:], dst.buf, q)
                    elif kind == "tm":
                        _, j0, n, post, dst = blk
                        for tt in range(4):
                            r2 = rr[0] % 2
                            rr[0] += 1
                            pb2 = ps[2 + r2]
                            for k in range(KC):
                                S.op("pe", lambda e: e.matmul(pb2[:, 0:n], xb[:, k, tt * 128:(tt + 1) * 128], Wb[:, k, j0:j0 + n],
                                                              start=(k == 0), stop=(k == KC - 1)),
                                     reads=[Wb, xb], writes=[pb2])
                            rows = slice(ch * 512 + tt * 128, ch * 512 + (tt + 1) * 128)
                            if post == "bf16":
                                o = tmb[r2]
                                S.op("act", lambda e: e.copy(o[:, 0:n], pb2[:, 0:n]), reads=[pb2], writes=[o])
                            elif post == "silu":
                                o = tmb[r2]
                                S.op("act", lambda e: e.activation(o[:, 0:n], pb2[:, 0:n], AF.Silu), reads=[pb2], writes=[o])
                            else:
                                o = tmo[r2]
                                S.op("act", lambda e: e.copy(o[:, 0:n], pb2[:, 0:n]), reads=[pb2], writes=[o])
                            S.dma("pool", dst[rows, :], o[:, 0:n], dst.buf, o)

        class V:
            def __init__(self, buf, ap):
                self.buf, self.ap = buf, ap

            def __getitem__(self, k):
                return self.ap[k]

        glist = []
        glist.append((0, 1024, [("rope", h * 128, V(P.qT, P.qT.ap[h])) for h in range(8)]))
        blocks = []
        for br in range(3):
            for g in range(2):
                blocks.append(("rope", ((br * 2 + 0) * 2 + g) * 128, V(P.kT, P.kT.ap[br * 2 + g])))
        for g in range(2):
            blocks.append(("fm", ((0 * 2 + 1) * 2 + g) * 128, V(P.vcT, P.vcT.ap[g])))
        blocks.append(("tm", 768, 256, "bf16", V(P.vs, P.vs.ap)))
        blocks.append(("tm", 1280, 256, "bf16", V(P.vw, P.vw.ap)))
        glist.append((1024, 1536, blocks))
        for gi in range(3):
            glist.append((2584 + gi * 1024, 1024,
                          [("gdn", j * 128, gi * 8 + j, V(P.gT, P.gT.ap[gi * 8 + j])) for j in range(8)]))
        glist.append((5672, 1024, [("tm", 0, 512, "silu", V(P.zs, P.zs.ap[:, 0:512])), ("tm", 512, 512, "silu", V(P.zs, P.zs.ap[:, 512:1024]))]))
        glist.append((2560, 24, [("tm", 0, 24, "f32", V(P.gate, P.gate.ap))]))
        glist.append((5656, 16, [("tm", 0, 16, "f32", V(P.bd, P.bd.ap))]))
        for gi_, (c0_, n_, bl_) in enumerate(glist):
            nxt = (glist[gi_ + 1][0], glist[gi_ + 1][1]) if gi_ + 1 < len(glist) else None
            group(c0_, n_, bl_, nxt)


def more_consts(S, P, c, st):
    c.eps = S.sbuf("c_eps", [128, 1], F32, st)
    S.op("pool", lambda e: e.memset(c.eps[:], EPS), writes=[c.eps])
    pi_ = S.sbuf("c_pi", [128, 1], I32, st)
    pf = S.sbuf("c_pf", [128, 1], F32, st)
    ge = S.sbuf("c_ge", [128, 1], F32, st)
    c.invf = S.sbuf("c_invf", [128, 1], F32, st)
    c.sgn = S.sbuf("c_sgn", [128, 1], F32, st)
    S.op("pool", lambda e: e.iota(pi_[:], [[0, 1]], base=0, channel_multiplier=1), writes=[pi_])
    S.op("dve", lambda e: e.tensor_copy(pf[:], pi_[:]), reads=[pi_], writes=[pf])
    S.op("dve", lambda e: e.tensor_scalar(ge[:], pf[:], 16.0, None, ALU.is_ge), reads=[pf], writes=[ge])
    S.op("dve", lambda e: e.tensor_scalar(c.sgn[:], ge[:], 2.0, -1.0, ALU.mult, ALU.add), reads=[ge], writes=[c.sgn])
    S.op("dve", lambda e: e.scalar_tensor_tensor(pf[:], ge[:], -16.0, pf[:], ALU.mult, ALU.add), reads=[ge, pf], writes=[pf])
    S.op("act", lambda e: e.activation(c.invf[:], pf[:], AF.Exp, scale=float(-2.0 * np.log(500000.0) / 32.0)), reads=[pf], writes=[c.invf])
    c.pf = pf
    c.one = S.sbuf("c_one", [128, 1], F32, st)
    S.op("pool", lambda e: e.memset(c.one[:], 1.0), writes=[c.one])
    c.zrow = S.sbuf("c_zrow", [1, 512], BF16, st)
    S.op("pool", lambda e: e.memset(c.zrow[:], 0.0), writes=[c.zrow])
    psw = S.sbuf("c_pswf", [128, 32], F32, st)
    c.pswap = S.sbuf("c_pswap", [128, 32], BF16, st)
    S.op("pool", lambda e: e.memset(psw[:], 0.0), writes=[psw])
    S.op("pool", lambda e: e.affine_select(psw[:, 0:16], psw[:, 0:16], [[-1, 16]], ALU.not_equal, 1.0, base=-16, channel_multiplier=1), reads=[psw], writes=[psw])
    S.op("pool", lambda e: e.affine_select(psw[:, 16:32], psw[:, 16:32], [[-1, 16]], ALU.not_equal, 1.0, base=0, channel_multiplier=1), reads=[psw], writes=[psw])
    S.op("dve", lambda e: e.tensor_copy(c.pswap[:], psw[:]), reads=[psw], writes=[c.pswap])
    c.anw = S.sbuf("c_anw", [128, KC], F32, st)
    S.dma("sp", c.anw[:], P.anw.ap, c.anw, P.anw)
    c.convw = S.sbuf("c_convw", [128, 24, 4], F32, st)
    S.dma("sp", c.convw[:], P.convw.ap, c.convw, P.convw)


def build(T, dbg=False, phases=("a0", "a1", "nsa", "gdn", "tail"), own_div=2):
    nc = bass.Bass("TRN2", target_bir_lowering=False)
    P = Prog()
    P.T, P.NT = T, T // 128
    st = ExitStack()
    with st:
        S = Sched(nc, st)
        sk = "ExternalOutput" if dbg else "Internal"
        P.x = S.dram("x", [T, D], F32, kind="ExternalInput")
        P.pos = S.dram("pos", [T], I32, kind="ExternalInput")
        P.anw = S.dram("anw", [128, KC], F32, kind="ExternalInput")
        P.w_in = S.dram("w_in", [D, 6696], F32, kind="ExternalInput")
        P.convw = S.dram("convw", [128, 24, 4], F32, kind="ExternalInput")
        P.cmp_wk = S.dram("cmp_wk", [32, 128, 128], F32, kind="ExternalInput")
        P.cmp_wv = S.dram("cmp_wv", [32, 128, 128], F32, kind="ExternalInput")
        P.cmp_pe = S.dram("cmp_pe", [128, 2, 32], F32, kind="ExternalInput")
        P.otm = S.dram("otm", [T, D], BF16, kind=sk)
        NO = P.NO = (T // 128) // own_div
        P.own_idx = S.dram("own_idx", [128, NO], I32, kind="ExternalInput")
        P.w_out = S.dram("w_out", [D, D], F32, kind="ExternalInput")
        P.fnw = S.dram("fnw", [D], F32, kind="ExternalInput")
        P.wr = S.dram("wr", [D, 72], F32, kind="ExternalInput")
        P.br = S.dram("br", [72], F32, kind="ExternalInput")
        P.wg = S.dram("wg", [64, D, 512], F32, kind="ExternalInput")
        P.wu = S.dram("wu", [64, D, 512], F32, kind="ExternalInput")
        P.wd = S.dram("wd", [64, 512, D], F32, kind="ExternalInput")
        P.moe_w = P.wg
        P.finw = S.dram("finw", [D], F32, kind="ExternalInput")
        P.h1 = S.dram("h1", [NO * 128, D], F32, kind=sk)
        P.xdisp = S.dram("xdisp", [64 * CAP, D], BF16, kind=sk)
        P.yall = S.dram("yall", [64 * CAP, D], F32, kind=sk)
        P.out = S.dram("out", [NO * 128, D], F32, kind="ExternalOutput")
        P.gpar = S.dram("gpar", [16], F32, kind="ExternalInput")
        P.gnw = S.dram("gnw", [128], F32, kind="ExternalInput")
        P.own_chunks = list(range(T // 512))
        P.xT = S.dram("xT", [128, KC, T], BF16, kind=sk)
        P.qT = S.dram("qT", [8, 128, T], BF16, kind=sk)
        P.kT = S.dram("kT", [6, 128, T], BF16, kind=sk)
        P.vcT = S.dram("vcT", [2, 128, T], BF16, kind=sk)
        P.vs = S.dram("vs", [T, 256], BF16, kind=sk)
        P.vw = S.dram("vw", [T, 256], BF16, kind=sk)
        P.gT = S.dram("gT", [24, 128, T], BF16, kind=sk)
        P.zs = S.dram("zs", [T, 1024], BF16, kind=sk)
        P.gate = S.dram("gate", [T, 24], F32, kind=sk)
        P.bd = S.dram("bd", [T, 16], F32, kind=sk)
        ps = [S.psum("ps%d" % i, [128, 512], F32, st) for i in range(8)]
        c = build_consts(S, P, st)
        more_consts(S, P, c, st)
        S.barrier()
        if "a0" in phases:
            with nc.named_scope("phase_a0"):
                phase_a0(S, P, c, ps)
            S.barrier()
        if "a1" in phases:
            with nc.named_scope("phase_a1"):
                phase_a1(S, P, c, ps)
            S.barrier()
        if "nsa" in phases:
            with nc.named_scope("phase_nsa"):
                phase_nsa(S, P, c, ps)
            S.barrier()
        if "gdn" in phases:
            with nc.named_scope("phase_gdn"):
                phase_gdn(S, P, c, ps)
            S.barrier()
        if "tail" in phases:
            R = Prog()
            R.w = S.sbuf("r_w", [128, NO, 2], F32, st)
            R.loc = S.sbuf("r_loc", [128, NO, 2], I32, st)
            with nc.named_scope("phase_tail1"):
                phase_tail1(S, P, c, ps, R)
            S.barrier()
            with nc.named_scope("phase_tail2"):
                phase_tail2(S, P, c, ps)
            S.barrier()
            with nc.named_scope("phase_tail3"):
                phase_tail3(S, P, c, ps, R)
            S.barrier()
        outs = [P.out, P.otm, P.xT, P.qT, P.kT, P.vcT, P.vs, P.vw, P.gT, P.zs, P.gate, P.bd]
        for e in ("sp", "pool", "act", "dve", "pe"):
            S.wait_all(e, outs)
        print("instructions:", S.n_ins, "dsems:", S.n_dsem)
    return nc


def phase_nsa(S, P, c, ps):
    T, NT = P.T, P.NT
    NCMP = T // 16 - 1
    NNT = (NCMP + 127) // 128
    NSLC = T // 64
    SC = float(HD ** -0.5)
    with ExitStack() as st:
        kT = [S.sbuf("n_kT%d" % i, [128, T], BF16, st) for i in range(6)]
        for i in range(6):
            S.dma("sp", kT[i][:], P.kT.ap[i], kT[i], P.kT)
        V1 = {}
        for bi, (nm, src) in enumerate((("s", P.vs), ("w", P.vw))):
            for g in range(2):
                v = S.sbuf("n_v%s%d" % (nm, g), [128, NT, 129], BF16, st)
                S.op("pool", lambda e: e.memset(v[:, :, 128:129], 1.0), writes=[v])
                S.dma("sp", v[:, :, 0:128], src.ap[:, g * 128:(g + 1) * 128].rearrange("(n p) d -> p n d", p=128), v, src)
                V1[(nm, g)] = v
        caus = S.sbuf("n_caus", [128, 128], BF16, st)
        caus2 = S.sbuf("n_caus2", [128, 128], BF16, st)
        cb_pre = [S.sbuf("n_cb%d" % nt, [128, T], BF16, st) for nt in range(NNT)]
        esel = S.sbuf("n_esel", [64, T], BF16, st)
        cbv = S.sbuf("n_cbv", [128, 2], F32, st)
        kcT = [S.sbuf("n_kcT%d" % g, [128, NNT * 128], BF16, st) for g in range(2)]
        rhsC = [S.sbuf("n_rhsC%d" % g, [128, NNT, 193], BF16, st) for g in range(2)]
        st2 = ExitStack()
        zf = S.sbuf("n_zf", [128, 128], F32, st2)
        S.op("pool", lambda e: e.memset(zf[:], 0.0), writes=[zf])
        S.op("pool", lambda e: e.affine_select(caus[:], zf[:], [[1, 128]], ALU.is_ge, NEG, base=0, channel_multiplier=-1), reads=[zf], writes=[caus])
        S.op("pool", lambda e: e.affine_select(caus2[:], zf[:], [[-1, 128]], ALU.is_gt, NEG, base=0, channel_multiplier=1), reads=[zf], writes=[caus2])
        zT = S.sbuf("n_zT", [128, T], BF16, st2)
        S.op("pool", lambda e: e.memset(zT[:], 0.0), writes=[zT])
        cbias = []
        for nt in range(NNT):
            cb = cb_pre[nt]
            S.op("pool", lambda e: e.affine_select(cb[:], zT[:], [[1, T]], ALU.is_ge, NEG, base=-(16 * nt * 128) - 31, channel_multiplier=-16),
                 reads=[zT], writes=[cb])
            cbias.append(cb)
        oneT = S.sbuf("n_oneT", [64, T], BF16, st2)
        S.op("pool", lambda e: e.memset(oneT[:], 1.0), writes=[oneT])
        S.op("pool", lambda e: e.affine_select(esel[:], oneT[:], [[1, T]], ALU.is_ge, 0.0, base=0, channel_multiplier=-64), reads=[oneT], writes=[esel])
        S.op("pool", lambda e: e.affine_select(esel[:], esel[:], [[-1, T]], ALU.is_ge, 0.0, base=63, channel_multiplier=64), reads=[esel], writes=[esel])
        wkf = S.sbuf("n_wkf", [128, 32, 128], F32, st2)
        wkb = [S.sbuf("n_wkb%d" % i, [128, 32, 128], BF16, st2) for i in range(2)]
        pef = S.sbuf("n_pef", [128, 2, 32], F32, st2)
        peb = S.sbuf("n_peb", [128, 2, 32], BF16, st2)
        S.dma("sp", pef[:], P.cmp_pe.ap, pef, P.cmp_pe)
        S.op("dve", lambda e: e.tensor_copy(peb[:], pef[:]), reads=[pef], writes=[peb])
        for i, src in enumerate((P.cmp_wk, P.cmp_wv)):
            S.dma("sp", wkf[:], src.ap.rearrange("l d e -> d l e"), wkf, src)
            S.op("dve", lambda e: e.tensor_copy(wkb[i][:], wkf[:]), reads=[wkf], writes=[wkb[i]])
        for i in range(2):
            pb = ps[6]
            for l in range(32):
                S.op("pe", lambda e: e.matmul(pb[:, 0:1], wkb[i][:, l, :], peb[:, i, l:l + 1], start=(l == 0), stop=(l == 31)),
                     reads=[wkb[i], peb], writes=[pb])
            S.op("act", lambda e: e.copy(cbv[:, i:i + 1], pb[:, 0:1]), reads=[pb], writes=[cbv])
        vcs = S.sbuf("n_vcs", [128, T], BF16, st2)
        vcTs = S.sbuf("n_vcTs", [128, NNT * 128], BF16, st2)
        for g in range(2):
            S.op("pool", lambda e: e.memset(kcT[g][:], 0.0), writes=[kcT[g]])
            S.op("pool", lambda e: e.memset(rhsC[g][:], 0.0), writes=[rhsC[g]])
            S.op("pool", lambda e: e.memset(rhsC[g][:, :, 128:129], 1.0), writes=[rhsC[g]])
            for nt in range(NNT):
                a = rhsC[g][:, nt, 129:129 + NSLC]
                S.op("pool", lambda e: e.memset(a, 1.0), writes=[rhsC[g]])
                S.op("pool", lambda e: e.affine_select(a, a, [[-4, NSLC]], ALU.is_ge, 0.0, base=nt * 128 + 1, channel_multiplier=1), reads=[rhsC[g]], writes=[rhsC[g]])
                S.op("pool", lambda e: e.affine_select(a, a, [[4, NSLC]], ALU.is_ge, 0.0, base=3 - nt * 128, channel_multiplier=-1), reads=[rhsC[g]], writes=[rhsC[g]])
        for g in range(2):
            S.dma("sp", vcs[:], P.vcT.ap[g], vcs, P.vcT)
            for i, (src, dst) in enumerate(((kT[0 * 2 + g], kcT[g]), (vcs, vcTs))):
                if i == 1:
                    S.op("pool", lambda e: e.memset(vcTs[:], 0.0), writes=[vcTs])
                for n0 in range(0, NCMP, 512):
                    nn = min(512, NCMP - n0)
                    pb = ps[6]
                    for l in range(32):
                        S.op("pe", lambda e: e.matmul(pb[:, 0:nn], wkb[i][:, l, :], src[:, 16 * n0 + l:16 * (n0 + nn - 1) + l + 1:16],
                                                      start=(l == 0), stop=(l == 31)),
                             reads=[wkb[i], src], writes=[pb])
                    S.op("act", lambda e: e.activation(dst[:, n0:n0 + nn], pb[:, 0:nn], AF.Identity, bias=cbv[:, i:i + 1]),
                         reads=[pb, cbv], writes=[dst])
            for nt in range(NNT):
                pb = ps[7]
                pv = pb.ap.bitcast(BF16)
                S.op("pe", lambda e: e.transpose(pv[:, 0:128], vcTs[:, nt * 128:(nt + 1) * 128], c.ident[:]), reads=[vcTs, c.ident], writes=[pb])
                S.op("act", lambda e: e.copy(rhsC[g][:, nt, 0:128], pv[:, 0:128]), reads=[pb], writes=[rhsC[g]])
            if NCMP % 128:
                pass
        S.barrier()
        st2.close()
        mbT = [S.sbuf("n_mbT%d" % g, [64, T], BF16, st) for g in range(2)]
        qs = [S.sbuf("n_q%d" % i, [128, 512], BF16, st) for i in range(2)]
        pT = [S.sbuf("n_pT%d" % i, [128, 512], BF16, st) for i in range(2)]
        oa = S.sbuf("n_oa", [128, 4, 1024], F32, st)
        oab = S.sbuf("n_oab", [128, 4, 1024], BF16, st)
        gt = S.sbuf("n_gt", [128, 4, 24], F32, st)
        imp = S.sbuf("n_imp", [128, 4, 64], F32, st)
        rl = S.sbuf("n_rl", [128, 8], F32, st)
        rls = [S.sbuf("n_rls%d" % i, [128, 2, 2], F32, st) for i in range(2)]
        ptmp = [S.sbuf("n_ptmp%d" % i, [128, 2, 128], F32, st) for i in range(2)]
        hbc = [0]
        tk = S.sbuf("n_tk", [128, 6, 64], F32, st)
        m8 = S.sbuf("n_m8", [128, 16], F32, st)
        mb = S.sbuf("n_mb", [128, 64], BF16, st)
        Dm = S.sbuf("n_Dm", [128, 64], F32, st)
        e0 = S.sbuf("n_e0", [128, 64], F32, st)
        di = S.sbuf("n_di", [128, 64], I32, st)
        S.op("pool", lambda e: e.iota(di[:], [[1, 64]], base=0, channel_multiplier=0), writes=[di])
        S.op("dve", lambda e: e.tensor_copy(Dm[:], di[:]), reads=[di], writes=[Dm])
        S.op("dve", lambda e: e.tensor_scalar(e0[:], Dm[:], 0.0, None, ALU.is_equal), reads=[Dm], writes=[e0])
        S.op("dve", lambda e: e.tensor_scalar(Dm[64:128, :], Dm[64:128, :], -1.0, None, ALU.add), reads=[Dm], writes=[Dm])
        cnt = [0]
        BIG = 1.0e4

        def run_items(items):
            n = len(items)
            for i in range(n + 1):
                if i < n:
                    items[i]["A"](ps[i % 2])
                if i >= 1:
                    it = items[i - 1]
                    it["B"](ps[(i - 1) % 2], pT[(i - 1) % 2])
                    it["C"](pT[(i - 1) % 2])
                    if it.get("post"):
                        it["post"]()

        qbuf = {}

        def load_q(h, tsl):
            q = qs[h % 2]
            S.dma("sp", q[:], P.qT.ap[h][:, tsl], q, P.qT)
            qbuf[h] = q

        for ch in P.own_chunks:
            q0 = ch * 4
            tsl = slice(ch * 512, (ch + 1) * 512)
            S.dma("sp", gt[:], P.gate.ap[tsl, :].rearrange("(n p) c -> p n c", p=128), gt, P.gate)
            S.op("act", lambda e: e.activation(gt[:], gt[:], AF.Sigmoid), reads=[gt], writes=[gt])
            accs = [(ps[2 + qi], 0) for qi in range(4)]
            for g in range(2):
                S.op("pool", lambda e: e.memset(imp[:], 0.0), writes=[imp])
                items = []
                for r in range(4):
                    h = g * 4 + r
                    hbc[0] += 1
                    hb = hbc[0]
                    for nt in range(NNT):
                        def A(sp, h=h, nt=nt, r=r):
                            if nt == 0:
                                if r == 0:
                                    load_q(h, tsl)
                                if r < 3:
                                    load_q(h + 1, tsl)
                            q = qbuf[h]
                            S.op("pe", lambda e: e.matmul(sp[:, :], kcT[g][:, nt * 128:(nt + 1) * 128], q[:], start=True, stop=False),
                                 reads=[kcT[g], q], writes=[sp])
                            S.op("pe", lambda e: e.matmul(sp[:, :], c.ident[:], cbias[nt][:, tsl], start=False, stop=True),
                                 reads=[c.ident, cbias[nt]], writes=[sp])

                        def B(sp, p):
                            S.op("act", lambda e: e.activation(p[:, :], sp[:, :], AF.Exp, scale=SC), reads=[sp], writes=[p])

                        def C(p, nt=nt, hb=hb):
                            bset = [ps[2 + 2 * (hb % 2)], ps[3 + 2 * (hb % 2)]]
                            if nt == 0:
                                for ab in bset:
                                    S.op("pe", lambda e: e.matmul(ab[:, 0:386], c.zrow[0:1, 0:128], c.zrow[0:1, 0:386], start=True, stop=False),
                                         reads=[c.zrow], writes=[ab])
                            for qi in range(4):
                                ab = bset[qi // 2]
                                off = (qi % 2) * 193
                                S.op("pe", lambda e: e.matmul(ab[:, off:off + 193], p[:, qi * 128:(qi + 1) * 128], rhsC[g][:, nt, :],
                                                              start=False, stop=(nt == NNT - 1 and qi % 2 == 1)),
                                     reads=[p, rhsC[g]], writes=[ab])

                        def post(h=h, hb=hb):
                            bset = [ps[2 + 2 * (hb % 2)], ps[3 + 2 * (hb % 2)]]
                            for bi, ab in enumerate(bset):
                                r_ = rls[bi]
                                v = ab[:, 0:386].rearrange("p (j w) -> p j w", j=2)
                                qsl = slice(bi * 2, bi * 2 + 2)
                                S.op("dve", lambda e: e.tensor_scalar(r_[:, :, 0:1], v[:, :, 128:129], 1e-30, None, ALU.max), reads=[ab], writes=[r_])
                                S.op("dve", lambda e: e.reciprocal(r_[:, :, 0:1], r_[:, :, 0:1]), reads=[r_], writes=[r_])
                                S.op("dve", lambda e: e.tensor_tensor(r_[:, :, 1:2], r_[:, :, 0:1], gt[:, qsl, h * 3:h * 3 + 1], ALU.mult), reads=[r_, gt], writes=[r_])
                                t_ = ptmp[bi]
                                S.op("dve", lambda e: e.tensor_tensor(t_[:, :, 0:64], v[:, :, 129:193], r_[:, :, 0:1].to_broadcast([128, 2, 64]), ALU.mult), reads=[ab, r_], writes=[t_])
                                S.op("pool", lambda e: e.tensor_tensor(imp[:, qsl, :], imp[:, qsl, :], t_[:, :, 0:64], ALU.add), reads=[imp, t_], writes=[imp])
                                S.op("dve", lambda e: e.tensor_tensor(oa[:, qsl, h * 128:(h + 1) * 128], v[:, :, 0:128], r_[:, :, 1:2].to_broadcast([128, 2, 128]), ALU.mult),
                                     reads=[ab, r_], writes=[oa])
                        items.append({"A": A, "B": B, "C": C, "post": post if nt == NNT - 1 else None})
                run_items(items)
                for qi in range(4):
                    qt = q0 + qi
                    if NSLC <= 16:
                        S.op("pool", lambda e: e.memset(mb[:], 0.0), writes=[mb])
                    else:
                        a, b_, ip, wk_, mk = tk[:, 0, :], tk[:, 1, :], tk[:, 2, :], tk[:, 3, :], tk[:, 4, :]
                        S.op("dve", lambda e: e.tensor_scalar(a, Dm[:], float(2 * qt - 1), None, ALU.is_ge), reads=[Dm], writes=[tk])
                        S.op("dve", lambda e: e.tensor_tensor(a, a, e0[:], ALU.max), reads=[tk, e0], writes=[tk])
                        S.op("dve", lambda e: e.scalar_tensor_tensor(ip, a, BIG, imp[:, qi, :], ALU.mult, ALU.add), reads=[tk, imp], writes=[tk])
                        S.op("dve", lambda e: e.tensor_scalar(b_, Dm[:], float(2 * qt), None, ALU.is_gt), reads=[Dm], writes=[tk])
                        S.op("dve", lambda e: e.scalar_tensor_tensor(ip, b_, -3.0 * BIG, ip, ALU.mult, ALU.add), reads=[tk], writes=[tk])
                        S.op("dve", lambda e: e.max(m8[:, 0:8], ip), reads=[tk], writes=[m8])
                        S.op("dve", lambda e: e.match_replace(wk_, m8[:, 0:8], ip, -1.0e9), reads=[tk, m8], writes=[tk])
                        S.op("dve", lambda e: e.max(m8[:, 8:16], wk_), reads=[tk], writes=[m8])
                        S.op("dve", lambda e: e.tensor_scalar(mk, ip, m8[:, 15:16], None, ALU.is_ge), reads=[tk, m8], writes=[tk])
                        S.op("dve", lambda e: e.tensor_scalar(mb[:], mk, -1.0, -NEG, ALU.add, ALU.mult), reads=[tk], writes=[mb])
                    pb = ps[6]
                    pv = pb.ap.bitcast(BF16)
                    S.op("pe", lambda e: e.transpose(pv[0:64, 0:128], mb[:], c.ident[:]), reads=[mb, c.ident], writes=[pb])
                    S.op("act", lambda e: e.copy(mbT[g][:, qt * 128:(qt + 1) * 128], pv[0:64, 0:128]), reads=[pb], writes=[mbT[g]])
                items = []
                for r in range(4):
                    h = g * 4 + r
                    for br, nm in ((1, "s"), (2, "w")):
                        kt_lo = 0 if br == 1 else max(0, q0 - 4)
                        kts = list(range(kt_lo, q0 + 4))
                        K_ = kT[br * 2 + g]
                        V_ = V1[(nm, g)]
                        hbc[0] += 1
                        hb = hbc[0]
                        for kt in kts:
                            qlo_t = max(kt, q0)
                            qhi_t = q0 + 3 if br == 1 else min(q0 + 3, kt + 4)
                            c_lo = (qlo_t - q0) * 128
                            c_hi = (qhi_t - q0 + 1) * 128

                            def A(sp, h=h, r=r, br=br, kt=kt, c_lo=c_lo, c_hi=c_hi, K_=K_, first=(br == 1 and kt == kts[0])):
                                if first:
                                    if r == 0:
                                        load_q(h, tsl)
                                    if r < 3:
                                        load_q(h + 1, tsl)
                                q = qbuf[h]
                                mm = [(slice(c_lo, c_hi), K_[:, kt * 128:(kt + 1) * 128], q[:, c_lo:c_hi], [K_, q])]
                                if kt >= q0:
                                    d0 = (kt - q0) * 128
                                    mm.append((slice(d0, d0 + 128), c.ident[:], caus[:], [c.ident, caus]))
                                if br == 2 and q0 <= kt + 4 <= q0 + 3:
                                    d0 = (kt + 4 - q0) * 128
                                    mm.append((slice(d0, d0 + 128), c.ident[:], caus2[:], [c.ident, caus2]))
                                if br == 1:
                                    mm.append((slice(c_lo, c_hi), esel[:, kt * 128:(kt + 1) * 128], mbT[g][:, ch * 512 + c_lo:ch * 512 + c_hi], [esel, mbT[g]]))
                                for mi, (osl, l_, r_, rd) in enumerate(mm):
                                    S.op("pe", lambda e: e.matmul(sp[:, osl], l_, r_, start=(mi == 0), stop=(mi == len(mm) - 1)), reads=rd, writes=[sp])

                            def B(sp, p, c_lo=c_lo, c_hi=c_hi):
                                S.op("act", lambda e: e.activation(p[:, c_lo:c_hi], sp[:, c_lo:c_hi], AF.Exp, scale=SC), reads=[sp], writes=[p])

                            def C(p, kt=kt, qlo_t=qlo_t, qhi_t=qhi_t, V_=V_, hb=hb, firstkt=(kt == kts[0])):
                                bset = [ps[2 + 2 * (hb % 2)], ps[3 + 2 * (hb % 2)]]
                                if firstkt:
                                    for ab in bset:
                                        S.op("pe", lambda e: e.matmul(ab[:, 0:258], c.zrow[0:1, 0:128], c.zrow[0:1, 0:258], start=True, stop=False),
                                             reads=[c.zrow], writes=[ab])
                                for qt in range(qlo_t, qhi_t + 1):
                                    qi = qt - q0
                                    ab = bset[qi // 2]
                                    off = (qi % 2) * 129
                                    S.op("pe", lambda e: e.matmul(ab[:, off:off + 129], p[:, qi * 128:(qi + 1) * 128], V_[:, kt, :],
                                                                  start=False, stop=(kt == qt and qi % 2 == 1)),
                                         reads=[p, V_], writes=[ab])

                            def post(h=h, br=br, hb=hb):
                                bset = [ps[2 + 2 * (hb % 2)], ps[3 + 2 * (hb % 2)]]
                                col = h * 3 + br
                                for bi, ab in enumerate(bset):
                                    r_ = rls[bi]
                                    v = ab[:, 0:258].rearrange("p (j w) -> p j w", j=2)
                                    qsl = slice(bi * 2, bi * 2 + 2)
                                    S.op("dve", lambda e: e.tensor_scalar(r_[:, :, 0:1], v[:, :, 128:129], 1e-30, None, ALU.max), reads=[ab], writes=[r_])
                                    S.op("dve", lambda e: e.reciprocal(r_[:, :, 0:1], r_[:, :, 0:1]), reads=[r_], writes=[r_])
                                    S.op("dve", lambda e: e.tensor_tensor(r_[:, :, 1:2], r_[:, :, 0:1], gt[:, qsl, col:col + 1], ALU.mult), reads=[r_, gt], writes=[r_])
                                    t_ = ptmp[bi]
                                    S.op("dve", lambda e: e.tensor_tensor(t_[:, :, :], v[:, :, 0:128], r_[:, :, 1:2].to_broadcast([128, 2, 128]), ALU.mult), reads=[ab, r_], writes=[t_])
                                    S.op("pool", lambda e: e.tensor_tensor(oa[:, qsl, h * 128:(h + 1) * 128], oa[:, qsl, h * 128:(h + 1) * 128], t_[:, :, :], ALU.add),
                                         reads=[oa, t_], writes=[oa])
                            items.append({"A": A, "B": B, "C": C, "post": post if kt == kts[-1] else None})
                run_items(items)
            S.op("act", lambda e: e.copy(oab[:], oa[:]), reads=[oa], writes=[oab])
            S.dma("pool", P.otm.ap[tsl, 0:1024].rearrange("(q p) c -> p q c", p=128), oab[:], P.otm, oab)


def phase_gdn(S, P, c, ps):
    T, NT = P.T, P.NT
    SCQ = float(HD ** -0.5)
    with ExitStack() as st:
        def mk(name):
            return S.sbuf(name, [128, 128], F32, st)
        ones = mk("g_ones"); Bd = mk("g_Bd"); LtriT = mk("g_LtriT"); RtriT = mk("g_RtriT"); U = mk("g_U"); LsT = mk("g_LsT")
        C0 = mk("g_C0"); C1 = mk("g_C1")
        S.op("pool", lambda e: e.memset(ones[:], 1.0), writes=[ones])
        S.op("pool", lambda e: e.memset(Bd[:], 1.0), writes=[Bd])
        S.op("pool", lambda e: e.memset(Bd[0:64, 64:128], 0.0), writes=[Bd])
        S.op("pool", lambda e: e.memset(Bd[64:128, 0:64], 0.0), writes=[Bd])
        S.op("pool", lambda e: e.memset(C0[:], 0.0), writes=[C0])
        S.op("pool", lambda e: e.memset(C0[0:64, :], 1.0), writes=[C0])
        S.op("pool", lambda e: e.memset(C1[:], 0.0), writes=[C1])
        S.op("pool", lambda e: e.memset(C1[64:128, :], 1.0), writes=[C1])
        S.op("pool", lambda e: e.affine_select(LtriT[:], Bd[:], [[1, 128]], ALU.is_ge, 0.0, base=0, channel_multiplier=-1), reads=[Bd], writes=[LtriT])
        S.op("pool", lambda e: e.affine_select(RtriT[:], Bd[:], [[-1, 128]], ALU.is_gt, 0.0, base=0, channel_multiplier=1), reads=[Bd], writes=[RtriT])
        S.op("pool", lambda e: e.affine_select(U[:], ones[:], [[-1, 128]], ALU.is_gt, 0.0, base=0, channel_multiplier=1), reads=[ones], writes=[U])
        S.op("pool", lambda e: e.affine_select(LsT[:], Bd[:], [[1, 128]], ALU.is_gt, 0.0, base=0, channel_multiplier=-1), reads=[Bd], writes=[LsT])
        nR = mk("g_nR"); nLs = mk("g_nLs")
        S.op("dve", lambda e: e.tensor_scalar(nR[:], RtriT[:], -1.0, None, ALU.mult), reads=[RtriT], writes=[nR])
        S.op("dve", lambda e: e.tensor_scalar(nLs[:], LsT[:], -1.0, None, ALU.mult), reads=[LsT], writes=[nLs])
        par = S.sbuf("g_par", [128, 16], F32, st)
        nw = S.sbuf("g_nw", [128, 128], F32, st)
        S.dma("sp", par[:], P.gpar.ap.partition_broadcast(128), par, P.gpar)
        S.dma("sp", nw[:], P.gnw.ap.partition_broadcast(128), nw, P.gnw)
        nexpA = S.sbuf("g_nexpA", [128, 8], F32, st)
        S.op("act", lambda e: e.activation(nexpA[:], par[:, 0:8], AF.Exp), reads=[par], writes=[nexpA])
        S.op("dve", lambda e: e.tensor_scalar(nexpA[:], nexpA[:], -1.0, None, ALU.mult), reads=[nexpA], writes=[nexpA])
        Sf = S.sbuf("g_Sf", [128, 8, 128], F32, st)
        Sb = S.sbuf("g_Sb", [128, 8, 128], BF16, st)
        S.op("pool", lambda e: e.memset(Sf[:], 0.0), writes=[Sf])
        S.op("pool", lambda e: e.memset(Sb[:], 0.0), writes=[Sb])
        gin = [S.sbuf("g_in%d" % i, [128, 24, 128], BF16, st) for i in range(2)]
        bdr = [S.sbuf("g_bd%d" % i, [128, 16], F32, st) for i in range(2)]
        tm = S.sbuf("g_tm", [128, 24, 128], BF16, st)
        junk = S.sbuf("g_junk", [128, 128], BF16, st)
        sq16 = S.sbuf("g_sq16", [128, 16, 128], F32, st)
        bsc = S.sbuf("g_bsc", [128, 4, 8], F32, st)
        E = S.sbuf("g_E", [128, 8, 2, 128], F32, st)
        tmpf = S.sbuf("g_tmpf", [128, 4, 128], F32, st)
        ssq = S.sbuf("g_ssq", [128, 16], F32, st)
        rn = S.sbuf("g_rn", [128, 16], F32, st)
        sc = S.sbuf("g_sc", [128, 8, 8], F32, st)
        gv = S.sbuf("g_gv", [128, 8], F32, st)
        egsL = [S.sbuf("g_egs%d" % i, [128, 32], F32, st) for i in range(2)]
        ssq2 = S.sbuf("g_ssq2", [128, 16], F32, st)
        rn2 = S.sbuf("g_rn2", [128, 16], F32, st)
        sq8 = S.sbuf("g_sq8", [128, 16, 128], F32, st)
        khat = S.sbuf("g_khat", [128, 8, 128], BF16, st)
        kb = S.sbuf("g_kb", [128, 8, 128], BF16, st)
        kbg = S.sbuf("g_kbg", [128, 8, 128], BF16, st)
        kdecL = [S.sbuf("g_kdec%d" % i, [128, 8, 128], BF16, st) for i in range(2)]
        qs_ = S.sbuf("g_qs", [128, 8, 128], BF16, st)
        qd = S.sbuf("g_qd", [128, 8, 128], BF16, st)
        vb = S.sbuf("g_vb", [128, 8, 128], BF16, st)
        khT = S.sbuf("g_khT", [128, 8, 128], BF16, st)
        kbT = S.sbuf("g_kbT", [128, 8, 128], BF16, st)
        qsT = S.sbuf("g_qsT", [128, 8, 128], BF16, st)
        qdTL = [S.sbuf("g_qdT%d" % i, [128, 8, 128], BF16, st) for i in range(2)]
        GU = [S.sbuf("g_GU%d" % i, [128, 128], F32, st) for i in range(2)]
        Pm = [S.sbuf("g_P%d" % i, [128, 8, 128], BF16, st) for i in range(2)]
        PTm = [S.sbuf("g_PT%d" % i, [128, 8, 128], BF16, st) for i in range(2)]
        Xm = [S.sbuf("g_X%d" % i, [128, 8, 128], BF16, st) for i in range(2)]
        atTL = [S.sbuf("g_atT%d" % i, [128, 8, 128], BF16, st) for i in range(2)]
        uL = [S.sbuf("g_u%d" % i, [128, 8, 128], F32, st) for i in range(2)]
        wTL = [S.sbuf("g_wT%d" % i, [128, 8, 128], BF16, st) for i in range(2)]
        vn = S.sbuf("g_vn", [128, 8, 128], BF16, st)
        o = S.sbuf("g_o", [128, 8, 128], F32, st)
        ob = S.sbuf("g_ob", [128, 1024], BF16, st)
        zt = S.sbuf("g_zt", [128, 1024], BF16, st)
        oTs = [S.sbuf("g_oT%d" % i, [128, 512], BF16, st) for i in range(2)]
        obc = S.sbuf("g_obc", [128, 4, 1024], BF16, st)
        rot = [0]

        def bank():
            rot[0] += 1
            return ps[rot[0] % 8]

        def load(i, b):
            S.dma("sp", gin[b][:], P.gT.ap[:, :, i * 128:(i + 1) * 128].rearrange("b p t -> p b t"), gin[b], P.gT)
            S.dma("sp", bdr[b][:], P.bd.ap[i * 128:(i + 1) * 128, :], bdr[b], P.bd)

        def transpose_group(dst, src, n, evac):
            for j0 in range(0, n, 4):
                pb = bank()
                pv = pb.ap.bitcast(BF16)
                for j in range(j0, min(n, j0 + 4)):
                    S.op("pe", lambda e: e.transpose(pv[:, (j - j0) * 128:(j - j0 + 1) * 128], src[:, j, :], c.ident[:]), reads=[src, c.ident], writes=[pb])
                nn = min(n, j0 + 4) - j0
                if evac == "act":
                    S.op("act", lambda e: e.copy(dst[:, j0:j0 + nn, :], pv[:, 0:nn * 128].rearrange("p (j t) -> p j t", j=nn)), reads=[pb], writes=[dst])
                else:
                    S.op("dve", lambda e: e.tensor_copy(dst[:, j0:j0 + nn, :], pv[:, 0:nn * 128].rearrange("p (j t) -> p j t", j=nn)), reads=[pb], writes=[dst])

        def prep_gen(i):
            b = i % 2
            own = (i // 4) in P.own_chunks
            egs = egsL[i % 2]; kdec = kdecL[i % 2]; qdT = qdTL[i % 2]; atT = atTL[i % 2]; u = uL[i % 2]; wT = wTL[i % 2]
            load(i, b)
            G = gin[b]
            transpose_group(tm, G, 24, "act")
            yield
            bd_ = bdr[b]
            S.op("act", lambda e: e.activation(sc[:, :, 0], bd_[:, 0:8], AF.Sigmoid), reads=[bd_], writes=[sc])
            S.op("dve", lambda e: e.tensor_tensor(gv[:], bd_[:, 8:16], par[:, 8:16], ALU.add), reads=[bd_, par], writes=[gv])
            S.op("act", lambda e: e.activation(gv[:], gv[:], AF.Exp), reads=[gv], writes=[gv])
            S.op("act", lambda e: e.activation(gv[:], gv[:], AF.Ln, bias=c.one[:, 0:1]), reads=[gv, c.one], writes=[gv])
            S.op("dve", lambda e: e.tensor_tensor(gv[:], gv[:], nexpA[:], ALU.mult), reads=[gv, nexpA], writes=[gv])
            pg = bank()
            for j, M_ in enumerate((LtriT, RtriT, C0, C1)):
                S.op("pe", lambda e: e.matmul(pg[:, j * 8:(j + 1) * 8], M_[:], gv[:], start=True, stop=True), reads=[M_, gv], writes=[pg])
            S.op("act", lambda e: e.activation(egs[:], pg[:, 0:32], AF.Exp), reads=[pg], writes=[egs])
            yield
            S.op("act", lambda e: e.activation(sq16[:], tm[:, 0:16, :], AF.Square), reads=[tm], writes=[sq16])
            S.op("dve", lambda e: e.tensor_reduce(ssq[:, 0:16], sq16[:], AX.X, ALU.add), reads=[sq16], writes=[ssq])
            S.op("act", lambda e: e.activation(rn[:], ssq[:], AF.Sqrt, bias=c.eps[:, 0:1]), reads=[ssq, c.eps], writes=[rn])
            S.op("dve", lambda e: e.reciprocal(rn[:], rn[:]), reads=[rn], writes=[rn])
            def bc(ap8):
                return ap8.unsqueeze(2).to_broadcast([128, 8, 128])
            beta8 = sc[:, :, 0]
            S.op("dve", lambda e: e.tensor_scalar(bsc[:, 0, :], beta8, -1.0, None, ALU.mult), reads=[sc], writes=[bsc])
            S.op("dve", lambda e: e.tensor_tensor(bsc[:, 1, :], beta8, egs[:, 0:8], ALU.mult), reads=[sc, egs], writes=[bsc])
            S.op("dve", lambda e: e.tensor_scalar(bsc[:, 2, :], rn[:, 0:8], SCQ, None, ALU.mult), reads=[rn], writes=[bsc])
            S.op("dve", lambda e: e.tensor_tensor(khat[:], tm[:, 8:16, :], bc(rn[:, 8:16]), ALU.mult), reads=[tm, rn], writes=[khat])
            S.op("dve", lambda e: e.tensor_tensor(kb[:], khat[:], bc(bsc[:, 0, :]), ALU.mult), reads=[khat, bsc], writes=[kb])
            S.op("pool", lambda e: e.tensor_tensor(kbg[:], khat[:], bc(bsc[:, 1, :]), ALU.mult), reads=[khat, bsc], writes=[kbg])
            S.op("pool", lambda e: e.tensor_tensor(kdec[:], khat[:], bc(egs[:, 8:16]), ALU.mult), reads=[khat, egs], writes=[kdec])
            S.op("dve", lambda e: e.tensor_tensor(qs_[:], tm[:, 0:8, :], bc(bsc[:, 2, :]), ALU.mult), reads=[tm, bsc], writes=[qs_])
            S.op("dve", lambda e: e.tensor_tensor(qd[:], qs_[:], bc(egs[:, 0:8]), ALU.mult), reads=[qs_, egs], writes=[qd])
            S.op("pool", lambda e: e.tensor_tensor(vb[:], tm[:, 16:24, :], bc(beta8), ALU.mult), reads=[tm, sc], writes=[vb])
            transpose_group(khT, khat, 8, "act")
            yield
            transpose_group(kbT, kb, 8, "act")
            yield
            transpose_group(qsT, qs_, 8, "act")
            yield
            transpose_group(qdT, qd, 8, "act")
            yield
            for h in range(8):
                gu = GU[h % 2]
                S.op("dve", lambda e: e.tensor_scalar(gu[:], U[:], gv[:, h:h + 1], None, ALU.mult), reads=[U, gv], writes=[gu])
                pd = bank()
                S.op("pe", lambda e: e.matmul(pd[:, 0:128], LtriT[:], gu[:], start=True, stop=True), reads=[LtriT, gu], writes=[pd])
                S.op("pe", lambda e: e.matmul(pd[:, 128:256], gu[:], LtriT[:], start=True, stop=True), reads=[LtriT, gu], writes=[pd])
                S.op("act", lambda e: e.activation(E[:, h, :, :], pd[:, 0:256].rearrange("p (a t) -> p a t", a=2), AF.Exp), reads=[pd], writes=[E])
                if h % 2 == 1:
                    yield
            P0, PT0, X0 = Pm[0], PTm[0], Xm[0]
            for hg in range(2):
                hs = range(hg * 4, hg * 4 + 4)
                pb = bank()
                for h in hs:
                    S.op("pe", lambda e: e.matmul(pb[:, (h % 4) * 128:(h % 4 + 1) * 128], kbT[:, h, :], khT[:, h, :], start=True, stop=True), reads=[kbT, khT], writes=[pb])
                S.op("dve", lambda e: e.tensor_tensor(tmpf[:], pb[:, :].rearrange("p (j t) -> p j t", j=4), E[:, hg * 4:hg * 4 + 4, 0, :], ALU.mult),
                     reads=[pb, E], writes=[tmpf])
                S.op("dve", lambda e: e.tensor_tensor(P0[:, hg * 4:hg * 4 + 4, :], tmpf[:], RtriT[:, :].unsqueeze(1).to_broadcast([128, 4, 128]), ALU.mult),
                     reads=[tmpf, RtriT], writes=[P0])
                pb = bank()
                for h in hs:
                    S.op("pe", lambda e: e.matmul(pb[:, (h % 4) * 128:(h % 4 + 1) * 128], khT[:, h, :], kbT[:, h, :], start=True, stop=True), reads=[kbT, khT], writes=[pb])
                S.op("dve", lambda e: e.tensor_tensor(tmpf[:], pb[:, :].rearrange("p (j t) -> p j t", j=4), E[:, hg * 4:hg * 4 + 4, 1, :], ALU.mult),
                     reads=[pb, E], writes=[tmpf])
                S.op("dve", lambda e: e.tensor_tensor(PT0[:, hg * 4:hg * 4 + 4, :], tmpf[:], LsT[:, :].unsqueeze(1).to_broadcast([128, 4, 128]), ALU.mult),
                     reads=[tmpf, LsT], writes=[PT0])
                pb = bank()
                for h in hs:
                    S.op("pe", lambda e: e.matmul(pb[:, (h % 4) * 128:(h % 4 + 1) * 128], khT[:, h, :], qsT[:, h, :], start=True, stop=True), reads=[qsT, khT], writes=[pb])
                S.op("dve", lambda e: e.tensor_tensor(tmpf[:], pb[:, :].rearrange("p (j t) -> p j t", j=4), E[:, hg * 4:hg * 4 + 4, 1, :], ALU.mult),
                     reads=[pb, E], writes=[tmpf])
                S.op("dve", lambda e: e.tensor_tensor(atT[:, hg * 4:hg * 4 + 4, :], tmpf[:], LtriT[:, :].unsqueeze(1).to_broadcast([128, 4, 128]), ALU.mult),
                     reads=[tmpf, LtriT], writes=[atT])
                S.op("pool", lambda e: e.tensor_tensor(X0[:, hg * 4:hg * 4 + 4, :], PT0[:, hg * 4:hg * 4 + 4, :],
                                                       c.ident[:, :].unsqueeze(1).to_broadcast([128, 4, 128]), ALU.add),
                     reads=[PT0, c.ident], writes=[X0])
                yield
            cur = 0
            for lev in range(1, 6):
                Pc, PTc, Xc = Pm[cur], PTm[cur], Xm[cur]
                Pn, PTn, Xn = Pm[1 - cur], PTm[1 - cur], Xm[1 - cur]
                for hg in range(2):
                    hs = range(hg * 4, hg * 4 + 4)
                    sl = slice(hg * 4, hg * 4 + 4)
                    pb = bank()
                    for h in hs:
                        S.op("pe", lambda e: e.matmul(pb[:, (h % 4) * 128:(h % 4 + 1) * 128], PTc[:, h, :], Pc[:, h, :], start=True, stop=True), reads=[PTc, Pc], writes=[pb])
                    S.op("act", lambda e: e.copy(Pn[:, sl, :], pb[:, :].rearrange("p (j t) -> p j t", j=4)), reads=[pb], writes=[Pn])
                    if lev < 5:
                        pb = bank()
                        for h in hs:
                            S.op("pe", lambda e: e.matmul(pb[:, (h % 4) * 128:(h % 4 + 1) * 128], Pc[:, h, :], PTc[:, h, :], start=True, stop=True), reads=[PTc, Pc], writes=[pb])
                        S.op("act", lambda e: e.copy(PTn[:, sl, :], pb[:, :].rearrange("p (j t) -> p j t", j=4)), reads=[pb], writes=[PTn])
                    pb = bank()
                    for h in hs:
                        S.op("pe", lambda e: e.matmul(pb[:, (h % 4) * 128:(h % 4 + 1) * 128], Pn[:, h, :], Xc[:, h, :], start=True, stop=True), reads=[Pn, Xc], writes=[pb])
                    S.op("dve", lambda e: e.tensor_tensor(Xn[:, sl, :], pb[:, :].rearrange("p (j t) -> p j t", j=4), Xc[:, sl, :], ALU.add), reads=[pb, Xc], writes=[Xn])
                    yield
                cur = 1 - cur
            X = Xm[cur]
            for hg in range(2):
                sl = slice(hg * 4, hg * 4 + 4)
                pb = bank()
                for h in range(hg * 4, hg * 4 + 4):
                    S.op("pe", lambda e: e.matmul(pb[:, (h % 4) * 128:(h % 4 + 1) * 128], X[:, h, :], vb[:, h, :], start=True, stop=True), reads=[X, vb], writes=[pb])
                S.op("act", lambda e: e.copy(u[:, sl, :], pb[:, :].rearrange("p (j t) -> p j t", j=4)), reads=[pb], writes=[u])
                pb = bank()
                for h in range(hg * 4, hg * 4 + 4):
                    S.op("pe", lambda e: e.matmul(pb[:, (h % 4) * 128:(h % 4 + 1) * 128], kbg[:, h, :], X[:, h, :], start=True, stop=True), reads=[X, kbg], writes=[pb])
                S.op("act", lambda e: e.copy(wT[:, sl, :], pb[:, :].rearrange("p (j t) -> p j t", j=4)), reads=[pb], writes=[wT])
                yield

        def scan_gen(i):
            b = i % 2
            own = (i // 4) in P.own_chunks
            egs = egsL[i % 2]; kdec = kdecL[i % 2]; qdT = qdTL[i % 2]; atT = atTL[i % 2]; u = uL[i % 2]; wT = wTL[i % 2]
            ssq = ssq2; rn = rn2; sq16 = sq8
            for cc in range(2):
                rs = slice(cc * 64, cc * 64 + 64)
                for hg in range(2):
                    sl = slice(hg * 4, hg * 4 + 4)
                    pb = bank()
                    for h in range(hg * 4, hg * 4 + 4):
                        S.op("pe", lambda e: e.matmul(pb[rs, (h % 4) * 128:(h % 4 + 1) * 128], wT[:, h, rs], Sb[:, h, :], start=True, stop=True), reads=[wT, Sb], writes=[pb])
                    S.op("dve", lambda e: e.tensor_tensor(vn[rs, sl, :], u[rs, sl, :], pb[rs, :].rearrange("p (j t) -> p j t", j=4), ALU.subtract), reads=[pb, u], writes=[vn])
                yield
                if own:
                    for hg in range(2):
                        sl = slice(hg * 4, hg * 4 + 4)
                        pb = bank()
                        for h in range(hg * 4, hg * 4 + 4):
                            cs = slice((h % 4) * 128, (h % 4 + 1) * 128)
                            S.op("pe", lambda e: e.matmul(pb[rs, cs], qdT[:, h, rs], Sb[:, h, :], start=True, stop=False), reads=[qdT, Sb], writes=[pb])
                            S.op("pe", lambda e: e.matmul(pb[rs, cs], atT[rs, h, rs], vn[rs, h, :], start=False, stop=True), reads=[atT, vn], writes=[pb])
                        S.op("act", lambda e: e.copy(o[rs, sl, :], pb[rs, :].rearrange("p (j t) -> p j t", j=4)), reads=[pb], writes=[o])
                    yield
                for hg in range(2):
                    sl = slice(hg * 4, hg * 4 + 4)
                    pb = bank()
                    for h in range(hg * 4, hg * 4 + 4):
                        S.op("pe", lambda e: e.matmul(pb[:, (h % 4) * 128:(h % 4 + 1) * 128], kdec[rs, h, :], vn[rs, h, :], start=True, stop=True), reads=[kdec, vn], writes=[pb])
                    S.op("dve", lambda e: e.tensor_tensor(Sf[:, sl, :], Sf[:, sl, :],
                                                          egs[:, 16 + cc * 8 + hg * 4:16 + cc * 8 + hg * 4 + 4].unsqueeze(2).to_broadcast([128, 4, 128]), ALU.mult),
                         reads=[Sf, egs], writes=[Sf])
                    S.op("dve", lambda e: e.tensor_tensor(Sf[:, sl, :], Sf[:, sl, :], pb[:, :].rearrange("p (j t) -> p j t", j=4), ALU.add),
                         reads=[Sf, pb], writes=[Sf])
                    S.op("act", lambda e: e.copy(Sb[:, sl, :], Sf[:, sl, :]), reads=[Sf], writes=[Sb])
                yield
            if own:
                S.dma("sp", zt[:], P.zs.ap[i * 128:(i + 1) * 128, :], zt, P.zs)
                S.op("act", lambda e: e.activation(sq16[:, 0:8, :], o[:], AF.Square), reads=[o], writes=[sq16])
                S.op("dve", lambda e: e.tensor_reduce(ssq[:, 0:8], sq16[:, 0:8, :], AX.X, ALU.add), reads=[sq16], writes=[ssq])
                S.op("act", lambda e: e.activation(rn[:, 0:8], ssq[:, 0:8], AF.Sqrt, bias=c.eps[:, 0:1], scale=1.0 / HD), reads=[ssq, c.eps], writes=[rn])
                S.op("dve", lambda e: e.reciprocal(rn[:, 0:8], rn[:, 0:8]), reads=[rn], writes=[rn])
                S.op("dve", lambda e: e.tensor_tensor(o[:], o[:], rn[:, 0:8].unsqueeze(2).to_broadcast([128, 8, 128]), ALU.mult), reads=[o, rn], writes=[o])
                S.op("pool", lambda e: e.tensor_tensor(o[:], o[:], nw[:, :].unsqueeze(1).to_broadcast([128, 8, 128]), ALU.mult), reads=[o, nw], writes=[o])
                qi = i % 4
                S.op("dve", lambda e: e.tensor_tensor(obc[:, qi, :], o[:, :, :].rearrange("p h d -> p (h d)"), zt[:], ALU.mult), reads=[o, zt], writes=[obc])
                if qi == 3:
                    ch = i // 4
                    S.dma("pool", P.otm.ap[ch * 512:(ch + 1) * 512, 1024:2048].rearrange("(q p) c -> p q c", p=128), obc[:], P.otm, obc)
            yield

        def drain(g):
            for _ in g:
                pass

        drain(prep_gen(0))
        for i in range(NT):
            sg = scan_gen(i)
            pg_ = prep_gen(i + 1) if i + 1 < NT else iter(())
            s_done = p_done = False
            while not (s_done and p_done):
                for _ in range(2):
                    if not p_done:
                        try:
                            next(pg_)
                        except StopIteration:
                            p_done = True
                if not s_done:
                    try:
                        next(sg)
                    except StopIteration:
                        s_done = True


CAP = 128


def own_tiles(P):
    return list(range(P.NO))


def phase_tail1(S, P, c, ps, R):
    tiles = own_tiles(P)
    NO = len(tiles)
    with ExitStack() as st:
        Wo = S.sbuf("t1_wo", [128, KC, D], BF16, st)
        wst = [S.sbuf("t1_wst%d" % i, [128, 2, D], F32, st) for i in range(2)]
        for k0 in range(0, KC, 2):
            w = wst[(k0 // 2) % 2]
            S.dma("sp", w[:], P.w_out.ap[k0 * 128:(k0 + 2) * 128, :].rearrange("(k p) n -> p k n", p=128), w, P.w_out)
            eng = ("dve", "pool")[(k0 // 2) % 2]
            S.op(eng, lambda e: e.tensor_copy(Wo[:, k0:k0 + 2, :], w[:]), reads=[w], writes=[Wo])
        fnw = S.sbuf("t1_fnw", [128, D], F32, st)
        S.dma("sp", fnw[:], P.fnw.ap.partition_broadcast(128), fnw, P.fnw)
        Wr = S.sbuf("t1_wr", [128, KC, 72], F32, st)
        S.dma("sp", Wr[:], P.wr.ap.rearrange("(k p) n -> p k n", p=128), Wr, P.wr)
        brb = S.sbuf("t1_br", [128, 72], F32, st)
        S.dma("sp", brb[:], P.br.ap.partition_broadcast(128), brb, P.br)
        tri = S.sbuf("t1_tri", [128, 128], F32, st)
        onesf = S.sbuf("t1_ones", [128, 128], F32, st)
        S.op("pool", lambda e: e.memset(onesf[:], 1.0), writes=[onesf])
        S.op("pool", lambda e: e.affine_select(tri[:], onesf[:], [[1, 128]], ALU.is_ge, 0.0, base=0, channel_multiplier=-1), reads=[onesf], writes=[tri])
        basei = S.sbuf("t1_basei", [128, 64], I32, st)
        base = S.sbuf("t1_base", [128, 64], F32, st)
        S.op("pool", lambda e: e.iota(basei[:], [[CAP, 64]], base=-1, channel_multiplier=0), writes=[basei])
        S.op("dve", lambda e: e.tensor_copy(base[:], basei[:]), reads=[basei], writes=[base])
        carry = S.sbuf("t1_carry", [128, 64], F32, st)
        S.op("pool", lambda e: e.memset(carry[:], 0.0), writes=[carry])
        zt = S.sbuf("t1_zt", [128, D], BF16, st)
        S.op("pool", lambda e: e.memset(zt[:], 0.0), writes=[zt])
        for r0 in range(0, 64 * CAP, 128):
            S.dma("pool", P.xdisp.ap[r0:r0 + 128, :], zt[:], P.xdisp, zt)
        otok = [S.sbuf("t1_otok%d" % i, [128, D], BF16, st) for i in range(2)]
        oTt = S.sbuf("t1_oTt", [128, KC, 128], BF16, st)
        idx = S.sbuf("t1_idx", [128, NO], I32, st)
        S.dma("sp", idx[:], P.own_idx.ap, idx, P.own_idx)
        xs = [S.sbuf("t1_x%d" % i, [128, D], F32, st) for i in range(2)]
        h1 = [S.sbuf("t1_h1%d" % i, [128, D], F32, st) for i in range(2)]
        hn = S.sbuf("t1_hn", [128, D], F32, st)
        hnb = [S.sbuf("t1_hnb%d" % i, [128, D], BF16, st) for i in range(2)]
        hT = S.sbuf("t1_hT", [128, KC, 128], F32, st)
        junk = S.sbuf("t1_junk", [128, D], BF16, st)
        ss = S.sbuf("t1_ss", [128, 4], F32, st)
        lgs = [S.sbuf("t1_lg%d" % i, [128, 72], F32, st) for i in range(2)]
        r8 = S.sbuf("t1_r8", [128, 12, 8], F32, st)
        m64 = S.sbuf("t1_m64", [128, 6, 64], F32, st)
        sm = S.sbuf("t1_sm", [128, 16], F32, st)
        loci = [S.sbuf("t1_loci%d_%d" % (i, k), [128, 1], I32, st) for i in range(2) for k in range(2)]

        def front(j):
            b = j % 2
            lg = lgs[b]
            X = xs[b]
            H = h1[b]
            OT = otok[b]
            def gather(jj):
                X_, OT_ = xs[jj % 2], otok[jj % 2]
                S.dma("pool", None, None, X_, P.x, extra_reads=[idx],
                      fn=lambda e: e.indirect_dma_start(X_[:, :], None, P.x.ap[:, :], bass.IndirectOffsetOnAxis(idx[:, jj:jj + 1], 0)))
                S.dma("pool", None, None, OT_, P.otm, extra_reads=[idx],
                      fn=lambda e: e.indirect_dma_start(OT_[:, :], None, P.otm.ap[:, :], bass.IndirectOffsetOnAxis(idx[:, jj:jj + 1], 0)))
            if j == 0:
                gather(0)
            if j + 1 < NO:
                gather(j + 1)
            for k0 in range(0, KC, 8):
                pb = ps[6 + (k0 // 8) % 2]
                pv = pb.ap.bitcast(BF16)
                for k in range(k0, k0 + 8):
                    S.op("pe", lambda e: e.transpose(pv[:, (k - k0) * 128:(k - k0 + 1) * 128], OT[:, k * 128:(k + 1) * 128], c.ident[:]), reads=[OT, c.ident], writes=[pb])
                S.op("act", lambda e: e.copy(oTt[:, k0:k0 + 8, :], pv[:, 0:1024].rearrange("p (k t) -> p k t", k=8)), reads=[pb], writes=[oTt])
            for nc4 in range(4):
                pb = ps[nc4 % 2]
                for k in range(KC):
                    S.op("pe", lambda e: e.matmul(pb[:, :], oTt[:, k, :], Wo[:, k, nc4 * 512:(nc4 + 1) * 512],
                                                  start=(k == 0), stop=(k == KC - 1)), reads=[oTt, Wo], writes=[pb])
                S.op("dve", lambda e: e.tensor_tensor(H[:, nc4 * 512:(nc4 + 1) * 512], pb[:, :], X[:, nc4 * 512:(nc4 + 1) * 512], ALU.add),
                     reads=[pb, X], writes=[H])
            S.dma("pool", P.h1.ap[j * 128:(j + 1) * 128, :], H[:], P.h1, H)
            yield
            S.op("act", lambda e: e.activation(junk[:], H[:], AF.Square, accum_out=ss[:, 0:1]), reads=[H], writes=[junk, ss])
            S.op("act", lambda e: e.activation(ss[:, 1:2], ss[:, 0:1], AF.Sqrt, bias=c.eps[:, 0:1], scale=1.0 / D), reads=[ss, c.eps], writes=[ss])
            S.op("dve", lambda e: e.reciprocal(ss[:, 1:2], ss[:, 1:2]), reads=[ss], writes=[ss])
            S.op("dve", lambda e: e.scalar_tensor_tensor(hn[:], H[:], ss[:, 1:2], fnw[:], ALU.mult, ALU.mult), reads=[H, ss, fnw], writes=[hn])
            HB = hnb[b]
            S.op("act", lambda e: e.copy(HB[:], hn[:]), reads=[hn], writes=[HB])
            yield
            for k0 in range(0, KC, 4):
                pb = ps[2 + (k0 // 4) % 2]
                for k in range(k0, k0 + 4):
                    S.op("pe", lambda e: e.transpose(pb[:, (k - k0) * 128:(k - k0 + 1) * 128], hn[:, k * 128:(k + 1) * 128], c.identf[:]),
                         reads=[hn, c.identf], writes=[pb])
                eng = ("act", "dve")[(k0 // 4) % 2]
                if eng == "act":
                    S.op("act", lambda e: e.copy(hT[:, k0:k0 + 4, :], pb[:, :].rearrange("p (k t) -> p k t", k=4)), reads=[pb], writes=[hT])
                else:
                    S.op("dve", lambda e: e.tensor_copy(hT[:, k0:k0 + 4, :], pb[:, :].rearrange("p (k t) -> p k t", k=4)), reads=[pb], writes=[hT])
            pl = ps[4]
            for k in range(KC):
                S.op("pe", lambda e: e.matmul(pl[:, 0:72], hT[:, k, :], Wr[:, k, :], start=(k == 0), stop=(k == KC - 1)), reads=[hT, Wr], writes=[pl])
            S.op("dve", lambda e: e.tensor_tensor(lg[:], pl[:, 0:72], brb[:], ALU.add), reads=[pl, brb], writes=[lg])
            yield

        def back(j):
            b = j % 2
            lg = lgs[b]
            HB = hnb[b]
            LG = lg[:, 0:8]
            LE = lg[:, 8:72]
            gmax, ngmax, se, pg, v12, w1, w2 = (sm[:, i:i + 1] for i in range(7))
            ohg, ex, ing, v8, sel1, sel2 = (r8[:, i, :] for i in range(6))
            S.op("dve", lambda e: e.reduce_max(gmax, LG, AX.X), reads=[lg], writes=[sm])
            S.op("dve", lambda e: e.tensor_scalar(ohg, LG, gmax, None, ALU.is_equal), reads=[lg, sm], writes=[r8])
            S.op("dve", lambda e: e.tensor_scalar(ngmax, gmax, -1.0, None, ALU.mult), reads=[sm], writes=[sm])
            S.op("act", lambda e: e.activation(ex, LG, AF.Exp, bias=ngmax, accum_out=se), reads=[lg, sm], writes=[r8, sm])
            S.op("dve", lambda e: e.reciprocal(pg, se), reads=[sm], writes=[sm])
            t64 = m64[:, 0, :]
            S.op("dve", lambda e: e.tensor_tensor(t64.rearrange("p (g j) -> p g j", g=8), LE.rearrange("p (g j) -> p g j", g=8),
                                                  ohg.unsqueeze(2).to_broadcast([128, 8, 8]), ALU.mult), reads=[lg, r8], writes=[m64])
            S.op("dve", lambda e: e.tensor_reduce(ing, t64.rearrange("p (g j) -> p j g", g=8), AX.X, ALU.add), reads=[m64], writes=[r8])
            S.op("dve", lambda e: e.max(v8, ing), reads=[r8], writes=[r8])
            S.op("dve", lambda e: e.tensor_scalar(sel1, ing, r8[:, 3, 0:1], None, ALU.is_equal), reads=[r8], writes=[r8])
            S.op("dve", lambda e: e.tensor_scalar(sel2, ing, r8[:, 3, 1:2], None, ALU.is_equal), reads=[r8], writes=[r8])
            S.op("dve", lambda e: e.tensor_tensor(v12, r8[:, 3, 0:1], r8[:, 3, 1:2], ALU.subtract), reads=[r8], writes=[sm])
            S.op("act", lambda e: e.activation(v12, v12, AF.Sigmoid), reads=[sm], writes=[sm])
            S.op("dve", lambda e: e.tensor_tensor(w1, v12, pg, ALU.mult), reads=[sm], writes=[sm])
            S.op("dve", lambda e: e.tensor_tensor(w2, pg, w1, ALU.subtract), reads=[sm], writes=[sm])
            S.op("dve", lambda e: e.tensor_copy(R.w[:, j, 0:1], w1), reads=[sm], writes=[R.w])
            S.op("dve", lambda e: e.tensor_copy(R.w[:, j, 1:2], w2), reads=[sm], writes=[R.w])
            yield
            m1, m2, mk_, val, tmp_ = (m64[:, i, :] for i in range(1, 6))
            for mm, sel in ((m1, sel1), (m2, sel2)):
                S.op("dve", lambda e: e.tensor_tensor(mm.rearrange("p (g j) -> p g j", g=8), ohg.unsqueeze(2).to_broadcast([128, 8, 8]),
                                                      sel.unsqueeze(1).to_broadcast([128, 8, 8]), ALU.mult), reads=[r8], writes=[m64])
            S.op("dve", lambda e: e.tensor_tensor(mk_, m1, m2, ALU.add), reads=[m64], writes=[m64])
            pc = ps[5]
            S.op("pe", lambda e: e.matmul(pc[:, 0:64], tri[:], mk_, start=True, stop=True), reads=[tri, m64], writes=[pc])
            S.op("pe", lambda e: e.matmul(pc[:, 64:128], onesf[:], mk_, start=True, stop=True), reads=[onesf, m64], writes=[pc])
            S.op("dve", lambda e: e.tensor_tensor(val, pc[:, 0:64], carry[:], ALU.add), reads=[pc, carry], writes=[m64])
            S.op("dve", lambda e: e.tensor_scalar(val, val, float(CAP), None, ALU.min), reads=[m64], writes=[m64])
            S.op("dve", lambda e: e.tensor_tensor(val, val, base[:], ALU.add), reads=[m64, base], writes=[m64])
            S.op("dve", lambda e: e.tensor_tensor(carry[:], carry[:], pc[:, 64:128], ALU.add), reads=[pc, carry], writes=[carry])
            yield
            for kk, mm in enumerate((m1, m2)):
                S.op("dve", lambda e: e.tensor_tensor(tmp_, mm, val, ALU.mult), reads=[m64], writes=[m64])
                S.op("dve", lambda e: e.reduce_sum(sm[:, 8 + kk:9 + kk], tmp_, AX.X), reads=[m64], writes=[sm])
                li = loci[b * 2 + kk]
                S.op("dve", lambda e: e.tensor_copy(li[:], sm[:, 8 + kk:9 + kk]), reads=[sm], writes=[li])
                S.op("dve", lambda e: e.tensor_copy(R.loc[:, j, kk:kk + 1], li[:]), reads=[li], writes=[R.loc])
                S.dma("pool", None, None, P.xdisp, HB, extra_reads=[li],
                      fn=lambda e: e.indirect_dma_start(P.xdisp.ap[:, :], bass.IndirectOffsetOnAxis(li[:, 0:1], 0), HB[:, :], None))
            yield

        def drain(g):
            for _ in g:
                pass

        drain(front(0))
        for j in range(NO):
            bg = back(j)
            fg = front(j + 1) if j + 1 < NO else iter(())
            b_done = f_done = False
            while not (b_done and f_done):
                if not f_done:
                    try:
                        next(fg)
                    except StopIteration:
                        f_done = True
                if not b_done:
                    try:
                        next(bg)
                    except StopIteration:
                        b_done = True


def phase_tail2(S, P, c, ps):
    with ExitStack() as st:
        wst = [S.sbuf("t2_wst%d" % i, [128, 8 * 512], F32, st) for i in range(5)]
        wbf = [S.sbuf("t2_wbf%d" % i, [128, 8 * 512], BF16, st) for i in range(5)]
        Xe = [S.sbuf("t2_xe%d" % i, [128, D], BF16, st) for i in range(2)]
        XT = S.sbuf("t2_xT", [128, KC, 128], BF16, st)
        hs = S.sbuf("t2_hs", [128, 512], F32, st)
        hb = S.sbuf("t2_hb", [128, 512], BF16, st)
        HT = S.sbuf("t2_hT", [128, 4, 128], BF16, st)
        ys = [S.sbuf("t2_y%d" % i, [128, D], F32, st) for i in range(2)]
        pcs = [0]

        def piece(src_ap):
            i = pcs[0] % 5
            eng = ("dve", "act")[pcs[0] % 2]
            pcs[0] += 1
            S.dma("sp", wst[i][:], src_ap, wst[i], P.moe_w)
            if eng == "act":
                S.op("act", lambda e: e.copy(wbf[i][:], wst[i][:]), reads=[wst[i]], writes=[wbf[i]])
            else:
                S.op(eng, lambda e: e.tensor_copy(wbf[i][:], wst[i][:]), reads=[wst[i]], writes=[wbf[i]])
            return wbf[i]

        for ex in range(64):
            X = Xe[ex % 2]
            S.dma("pool", X[:], P.xdisp.ap[ex * CAP:(ex + 1) * CAP, :], X, P.xdisp)
            for k0 in range(0, KC, 8):
                pb = ps[6 + (k0 // 8) % 2]
                pv = pb.ap.bitcast(BF16)
                for k in range(k0, k0 + 8):
                    S.op("pe", lambda e: e.transpose(pv[:, (k - k0) * 128:(k - k0 + 1) * 128], X[:, k * 128:(k + 1) * 128], c.ident[:]), reads=[X, c.ident], writes=[pb])
                S.op("act", lambda e: e.copy(XT[:, k0:k0 + 8, :], pv[:, 0:1024].rearrange("p (k t) -> p k t", k=8)), reads=[pb], writes=[XT])
            for wi, (W_, pb) in enumerate(((P.wg, ps[0]), (P.wu, ps[1]))):
                for half in range(2):
                    wb = piece(W_.ap[ex][half * 1024:(half + 1) * 1024, :].rearrange("(k p) n -> p k n", p=128))
                    for k in range(8):
                        kk = half * 8 + k
                        S.op("pe", lambda e: e.matmul(pb[:, :], XT[:, kk, :], wb[:, k * 512:(k + 1) * 512], start=(kk == 0), stop=(kk == KC - 1)),
                             reads=[XT, wb], writes=[pb])
            S.op("act", lambda e: e.activation(hs[:], ps[0][:, :], AF.Silu), reads=[ps[0]], writes=[hs])
            S.op("dve", lambda e: e.tensor_tensor(hb[:], hs[:], ps[1][:, :], ALU.mult), reads=[hs, ps[1]], writes=[hb])
            pb = ps[6]
            pv = pb.ap.bitcast(BF16)
            for k in range(4):
                S.op("pe", lambda e: e.transpose(pv[:, k * 128:(k + 1) * 128], hb[:, k * 128:(k + 1) * 128], c.ident[:]), reads=[hb, c.ident], writes=[pb])
            S.op("act", lambda e: e.copy(HT[:, :, :], pv[:, 0:512].rearrange("p (k t) -> p k t", k=4)), reads=[pb], writes=[HT])
            Y = ys[ex % 2]
            for half in range(2):
                wb = piece(P.wd.ap[ex][half * 256:(half + 1) * 256, :].rearrange("(k p) n -> p k n", p=128))
                for k in range(2):
                    kk = half * 2 + k
                    for n4 in range(4):
                        pby = ps[2 + n4]
                        S.op("pe", lambda e: e.matmul(pby[:, :], HT[:, kk, :], wb[:, k * 2048 + n4 * 512:k * 2048 + (n4 + 1) * 512], start=(kk == 0), stop=(kk == 3)),
                             reads=[HT, wb], writes=[pby])
            for n4 in range(4):
                if n4 % 2 == 0:
                    S.op("act", lambda e: e.copy(Y[:, n4 * 512:(n4 + 1) * 512], ps[2 + n4][:, :]), reads=[ps[2 + n4]], writes=[Y])
                else:
                    S.op("dve", lambda e: e.tensor_copy(Y[:, n4 * 512:(n4 + 1) * 512], ps[2 + n4][:, :]), reads=[ps[2 + n4]], writes=[Y])
            S.dma("pool", P.yall.ap[ex * CAP:(ex + 1) * CAP, :], Y[:], P.yall, Y)


def phase_tail3(S, P, c, ps, R):
    tiles = own_tiles(P)
    with ExitStack() as st:
        fw = S.sbuf("t3_fw", [128, D], F32, st)
        S.dma("sp", fw[:], P.finw.ap.partition_broadcast(128), fw, P.finw)
        h1 = [S.sbuf("t3_h%d" % i, [128, D], F32, st) for i in range(2)]
        y1 = [S.sbuf("t3_y1%d" % i, [128, D], F32, st) for i in range(2)]
        y2 = [S.sbuf("t3_y2%d" % i, [128, D], F32, st) for i in range(2)]
        ot = [S.sbuf("t3_o%d" % i, [128, D], F32, st) for i in range(2)]
        junk = S.sbuf("t3_junk", [128, D], BF16, st)
        ss = S.sbuf("t3_ss", [128, 2], F32, st)
        for j in range(len(tiles)):
            b = j % 2
            H, Y1, Y2, O = h1[b], y1[b], y2[b], ot[b]
            S.dma("sp", H[:], P.h1.ap[j * 128:(j + 1) * 128, :], H, P.h1)
            for kk, Y in enumerate((Y1, Y2)):
                S.dma("pool", None, None, Y, P.yall, extra_reads=[R.loc],
                      fn=lambda e: e.indirect_dma_start(Y[:, :], None, P.yall.ap[:, :], bass.IndirectOffsetOnAxis(R.loc[:, j, kk:kk + 1], 0)))
            S.op("dve", lambda e: e.scalar_tensor_tensor(H[:], Y1[:], R.w[:, j, 0:1], H[:], ALU.mult, ALU.add), reads=[Y1, R.w, H], writes=[H])
            S.op("dve", lambda e: e.scalar_tensor_tensor(H[:], Y2[:], R.w[:, j, 1:2], H[:], ALU.mult, ALU.add), reads=[Y2, R.w, H], writes=[H])
            S.op("act", lambda e: e.activation(junk[:], H[:], AF.Square, accum_out=ss[:, 0:1]), reads=[H], writes=[junk, ss])
            S.op("act", lambda e: e.activation(ss[:, 1:2], ss[:, 0:1], AF.Sqrt, bias=c.eps[:, 0:1], scale=1.0 / D), reads=[ss, c.eps], writes=[ss])
            S.op("dve", lambda e: e.reciprocal(ss[:, 1:2], ss[:, 1:2]), reads=[ss], writes=[ss])
            S.op("dve", lambda e: e.scalar_tensor_tensor(O[:], H[:], ss[:, 1:2], fw[:], ALU.mult, ALU.mult), reads=[H, ss, fw], writes=[O])
            S.dma("sp", P.out.ap[j * 128:(j + 1) * 128, :], O[:], P.out, O)


def make_in_maps(inputs, T, n_batch, own_div=2):
    f = lambda a: np.ascontiguousarray(np.asarray(a))
    l = 0
    shared = {
        "anw": f(np.asarray(inputs["attn_norm_w"])[l].reshape(KC, 128).T),
        "w_in": f(np.asarray(inputs["w_in"])[l]),
        "convw": f(np.asarray(inputs["gdn_conv_w"])[l].reshape(4, 24, 128).transpose(2, 1, 0)),
        "cmp_wk": f(np.asarray(inputs["cmp_wk"])[l]),
        "cmp_wv": f(np.asarray(inputs["cmp_wv"])[l]),
        "cmp_pe": f(np.stack([np.asarray(inputs["cmp_pek"])[l].T, np.asarray(inputs["cmp_pev"])[l].T], axis=1)),
        "gpar": f(np.concatenate([np.asarray(inputs["gdn_a_log"])[l], np.asarray(inputs["gdn_dt_bias"])[l]])),
        "gnw": f(np.asarray(inputs["gdn_norm_w"])[l]),
        "w_out": f(np.asarray(inputs["w_out"])[l]),
        "fnw": f(np.asarray(inputs["ffn_norm_w"])[l]),
        "wr": f(np.concatenate([np.asarray(inputs["router_group_w"])[l], np.asarray(inputs["router_expert_w"])[l]], axis=1)),
        "br": f(np.concatenate([np.asarray(inputs["router_group_b"])[l], np.asarray(inputs["router_expert_b"])[l]])),
        "wg": f(np.asarray(inputs["moe_w_gate"])[l]),
        "wu": f(np.asarray(inputs["moe_w_up"])[l]),
        "wd": f(np.asarray(inputs["moe_w_down"])[l]),
        "finw": f(np.asarray(inputs["final_norm_w"])),
    }
    maps, owns = [], []
    nch = T // 512
    for b in range(n_batch):
        for g in range(own_div):
            chunks = [ch for ch in range(nch) if ch % own_div == g]
            idx = np.concatenate([np.arange(ch * 512, (ch + 1) * 512) for ch in chunks]).astype(np.int32)
            m = dict(shared)
            m["x"] = f(np.asarray(inputs["x"])[b, :T])
            m["pos"] = f(np.asarray(inputs["positions"])[b, :T].astype(np.int32))
            m["own_idx"] = f(idx.reshape(-1, 128).T)
            maps.append(m)
            owns.append((b, idx))
    return maps, owns


_NC_CACHE = {}


def kernel(**inputs):
    T = 4096
    B = 4
    if "nc" not in _NC_CACHE:
        _NC_CACHE["nc"] = build(T)
    nc = _NC_CACHE["nc"]
    maps, owns = make_in_maps(inputs, T, B)
    res = run_bass_kernel_spmd(nc, maps, core_ids=list(range(8)))
    out = np.zeros((B, T, D), np.float32)
    for (b, idx), r in zip(owns, res.results):
        out[b, idx] = r["out"]
    return out
```
